# Optimizing a Trainium2 kernel written in Bass

```python
import math
import jax, jax.numpy as jnp
from jax import lax
import numpy as np

D_MODEL = 2048
BATCH = 4
SEQ = 2048
DEPTH = 2

GRID_W = 64
CTX_LEN = 256
NORM_EPS = 1e-6

N_DIFF_HEADS = 8
HEAD_DIM = 64
V_HEAD_DIM = 2 * HEAD_DIM
QK_W = N_DIFF_HEADS * 2 * HEAD_DIM
ATTN_W = N_DIFF_HEADS * V_HEAD_DIM
Q_BLOCK = 128
ROPE_BASE = 10000.0
ROPE_AXIS_DIM = HEAD_DIM // 2
ROPE_FREQS = ROPE_AXIS_DIM // 2
DIFF_SUBLN_EPS = 1e-5

SC_W = D_MODEL // 4
SC_CONV_WIDTH = 3

CF_W = D_MODEL // 4
CF_CONV_WIDTH = 31
CF_LN_EPS = 1e-5

N_BRANCHES = 3

OFF_Q = 0
OFF_K = OFF_Q + QK_W
OFF_V = OFF_K + QK_W
OFF_SC = OFF_V + ATTN_W
OFF_CF = OFF_SC + 3 * SC_W
OFF_GATE = OFF_CF + 2 * CF_W
C_TOT = OFF_GATE + N_BRANCHES * D_MODEL

N_GROUPS = 4
EXPERTS_PER_GROUP = 8
N_EXPERTS = N_GROUPS * EXPERTS_PER_GROUP
TOP_K = 2
EXPERT_HIDDEN = D_MODEL // 2
MOE_BLOCK = 128

kernel_name = "hybrid_diffattn_conv_hmoe_dit_block"


def rmsnorm(x, g, eps=NORM_EPS):
    xf = x.astype(jnp.float32)
    y = xf * lax.rsqrt(jnp.mean(xf * xf, axis=-1, keepdims=True) + eps)
    return (y * g.astype(jnp.float32)).astype(x.dtype)


def layernorm(x, g, b, eps=CF_LN_EPS):
    xf = x.astype(jnp.float32)
    mu = jnp.mean(xf, axis=-1, keepdims=True)
    var = jnp.mean(jnp.square(xf - mu), axis=-1, keepdims=True)
    y = (xf - mu) * lax.rsqrt(var + eps)
    return (y * g.astype(jnp.float32) + b.astype(jnp.float32)).astype(x.dtype)


def modulate(x, shift, scale):
    return x * (1 + scale) + shift


def depthwise_conv_centred(x, w):
    k = w.shape[0]
    return lax.conv_general_dilated(
        x, w[:, None, :].astype(x.dtype), window_strides=(1,),
        padding=[(k // 2, k // 2)], dimension_numbers=("NWC", "WIO", "NWC"),
        feature_group_count=x.shape[-1])


def axial_rope_tables(n_tokens):
    rows = n_tokens // GRID_W
    row = jnp.repeat(jnp.arange(rows, dtype=jnp.int32), GRID_W)
    col = jnp.tile(jnp.arange(GRID_W, dtype=jnp.int32), rows)
    pos = jnp.stack([row, col], axis=-1).astype(jnp.float32)
    inv_freq = ROPE_BASE ** (-jnp.arange(ROPE_FREQS, dtype=jnp.float32) / ROPE_FREQS)
    ang = pos[:, :, None] * inv_freq
    return jnp.cos(ang), jnp.sin(ang)


def apply_axial_rope(x, cos, sin):
    xr = x.reshape(x.shape[:-1] + (2, 2, ROPE_FREQS))
    x1, x2 = xr[..., 0, :], xr[..., 1, :]
    cos = cos.astype(x.dtype)
    sin = sin.astype(x.dtype)
    out = jnp.stack([x1 * cos - x2 * sin, x1 * sin + x2 * cos], axis=-2)
    return out.reshape(x.shape)


def split_qk(p):
    b, l, _ = p.shape
    return p.reshape(b, l, N_DIFF_HEADS, 2, HEAD_DIM).transpose(0, 2, 3, 1, 4)


def split_v(p):
    b, l, _ = p.shape
    return p.reshape(b, l, N_DIFF_HEADS, V_HEAD_DIM).transpose(0, 2, 1, 3)


def diff_attention(q, k, v, lam):
    b, h, _, lq, d = q.shape
    nb = lq // Q_BLOCK
    q_blocks = jnp.moveaxis(q.reshape(b, h, 2, nb, Q_BLOCK, d), 3, 0)
    scale = HEAD_DIM ** -0.5

    def one_block(qb):
        s = jnp.einsum("bhmqd,bhmkd->bhmqk", qb, k).astype(jnp.float32) * scale
        p = jax.nn.softmax(s, axis=-1)
        a = p[:, :, 0] - lam * p[:, :, 1]
        return jnp.einsum("bhqk,bhkv->bhqv", a.astype(v.dtype), v)

    o = lax.map(one_block, q_blocks)
    return jnp.moveaxis(o, 0, 2).reshape(b, h, lq, V_HEAD_DIM)


def diff_heads_out(o, subln_g, lam_init):
    o = rmsnorm(o, subln_g, DIFF_SUBLN_EPS) * (1.0 - lam_init)
    b, h, l, dv = o.shape
    return o.transpose(0, 2, 1, 3).reshape(b, l, h * dv)


def short_conv_mixer(p, conv_w):
    b_gate, c_gate, x_in = jnp.split(p, 3, axis=-1)
    return b_gate * depthwise_conv_centred(c_gate * x_in, conv_w)


def conformer_conv(p, dw_w, dw_b, ln_g, ln_b):
    a, g = jnp.split(p, 2, axis=-1)
    z = depthwise_conv_centred(a * jax.nn.sigmoid(g), dw_w) + dw_b.astype(p.dtype)
    return jax.nn.silu(layernorm(z, ln_g, ln_b))


def merge_branches(p, attn_flat, w_attn_out, sc_conv_w, w_sc_out, cf_dw_w, cf_dw_b,
                   cf_ln_g, cf_ln_b, w_cf_out, b_gate, w_mix):
    y_a = attn_flat @ w_attn_out
    y_b = short_conv_mixer(p[..., OFF_SC:OFF_CF], sc_conv_w) @ w_sc_out
    y_c = conformer_conv(p[..., OFF_CF:OFF_GATE], cf_dw_w, cf_dw_b, cf_ln_g, cf_ln_b) @ w_cf_out
    g_a, g_b, g_c = jnp.split(jax.nn.sigmoid(p[..., OFF_GATE:] + b_gate), N_BRANCHES, axis=-1)
    return (g_a * y_a + g_b * y_b + g_c * y_c) @ w_mix


def hier_moe(tokens, layer, rg_w, rg_b, re_w, re_b, exp_w_gu, exp_w_down):
    n_tok = tokens.shape[0]
    lg = (tokens @ rg_w).astype(jnp.float32) + rg_b.astype(jnp.float32)
    grp = jnp.argmax(lg, axis=-1).astype(jnp.int32)
    p_grp = jnp.take_along_axis(jax.nn.softmax(lg, axis=-1), grp[:, None], axis=-1)
    le = ((tokens @ re_w).astype(jnp.float32) + re_b.astype(jnp.float32)).reshape(
        n_tok, N_GROUPS, EXPERTS_PER_GROUP)
    le = jnp.take_along_axis(le, grp[:, None, None], axis=1)[:, 0]
    top_p, top_i = lax.top_k(jax.nn.softmax(le, axis=-1), TOP_K)
    weights = p_grp * top_p / jnp.sum(top_p, axis=-1, keepdims=True)
    expert_id = grp[:, None] * EXPERTS_PER_GROUP + top_i.astype(jnp.int32)

    n_slot = n_tok * TOP_K
    e_flat = expert_id.reshape(n_slot)
    w_flat = weights.reshape(n_slot)
    tok_flat = jnp.arange(n_slot, dtype=jnp.int32) // TOP_K
    order = jnp.argsort(e_flat)
    e_sorted = e_flat[order]
    counts = jnp.zeros((N_EXPERTS,), jnp.int32).at[e_flat].add(1)
    padded = (counts + MOE_BLOCK - 1) // MOE_BLOCK * MOE_BLOCK
    start = jnp.cumsum(counts) - counts
    pad_end = jnp.cumsum(padded)
    pad_start = pad_end - padded
    dest = pad_start[e_sorted] + jnp.arange(n_slot, dtype=jnp.int32) - start[e_sorted]
    n_rows = -(-n_slot // MOE_BLOCK) * MOE_BLOCK + N_EXPERTS * MOE_BLOCK
    n_blocks = n_rows // MOE_BLOCK
    row_tok = jnp.zeros((n_rows,), jnp.int32).at[dest].set(tok_flat[order])
    row_w = jnp.zeros((n_rows,), jnp.float32).at[dest].set(w_flat[order])
    blk_start = jnp.arange(n_blocks, dtype=jnp.int32) * MOE_BLOCK
    blk_exp = jnp.minimum(jnp.searchsorted(pad_end, blk_start, side="right"), N_EXPERTS - 1)
    x_blocks = tokens[row_tok].reshape(n_blocks, MOE_BLOCK, tokens.shape[-1])

    def expert_block(args):
        xb, e = args
        g, u = jnp.split(xb @ exp_w_gu[layer, e], 2, axis=-1)
        return (jax.nn.silu(g) * u) @ exp_w_down[layer, e]

    y = lax.map(expert_block, (x_blocks, blk_exp)).reshape(n_rows, tokens.shape[-1])
    y = y * row_w[:, None].astype(y.dtype)
    return jnp.zeros_like(tokens).at[row_tok].add(y)


def setup_inputs(seed: int = 0) -> dict:
    key = jax.random.key(seed)
    keys = jax.random.split(key, 32)
    counter = [0]
    f32 = jnp.float32
    D, L = D_MODEL, DEPTH

    def nrm(shape, scale):
        k = keys[counter[0]]
        counter[0] += 1
        return jax.random.normal(k, shape, f32) * scale

    def gain(shape):
        return 1.0 + nrm(shape, 0.05)

    return {
        "x": nrm((BATCH, SEQ, D), 1.0),
        "c": nrm((BATCH, D), 1.0),
        "ctx": nrm((BATCH, CTX_LEN, D), 1.0),
        "c_ctx": nrm((D,), 1.0),
        "ada_w": nrm((L, D, 6 * D), 0.5 * D ** -0.5),
        "ada_b": nrm((L, 6 * D), 0.02),
        "norm1_g": gain((L, D)),
        "w_in": nrm((L, D, C_TOT), D ** -0.5),
        "b_gate": nrm((L, N_BRANCHES * D), 0.02),
        "diff_lambda": nrm((L, 4, HEAD_DIM), 0.1),
        "subln_g": gain((L, V_HEAD_DIM)),
        "w_attn_out": nrm((L, ATTN_W, D), ATTN_W ** -0.5),
        "sc_conv_w": nrm((L, SC_CONV_WIDTH, SC_W), SC_CONV_WIDTH ** -0.5),
        "w_sc_out": nrm((L, SC_W, D), SC_W ** -0.5),
        "cf_dw_w": nrm((L, CF_CONV_WIDTH, CF_W), CF_CONV_WIDTH ** -0.5),
        "cf_dw_b": nrm((L, CF_W), 0.02),
        "cf_ln_g": gain((L, CF_W)),
        "cf_ln_b": nrm((L, CF_W), 0.02),
        "w_cf_out": nrm((L, CF_W, D), CF_W ** -0.5),
        "w_mix": nrm((L, D, D), D ** -0.5),
        "norm2_g": gain((L, D)),
        "router_g_w": nrm((L, D, N_GROUPS), D ** -0.5),
        "router_g_b": nrm((L, N_GROUPS), 0.01),
        "router_e_w": nrm((L, D, N_EXPERTS), D ** -0.5),
        "router_e_b": nrm((L, N_EXPERTS), 0.01),
        "exp_w_gu": nrm((L, N_EXPERTS, D, 2 * EXPERT_HIDDEN), D ** -0.5),
        "exp_w_down": nrm((L, N_EXPERTS, EXPERT_HIDDEN, D), EXPERT_HIDDEN ** -0.5),
        "final_g": gain((D,)),
    }


def reference(x, c, ctx, c_ctx, ada_w, ada_b, norm1_g, w_in, b_gate, diff_lambda, subln_g,
              w_attn_out, sc_conv_w, w_sc_out, cf_dw_w, cf_dw_b, cf_ln_g, cf_ln_b, w_cf_out,
              w_mix, norm2_g, router_g_w, router_g_b, router_e_w, router_e_b, exp_w_gu,
              exp_w_down, final_g):
    bsz, n_lat, d = x.shape
    n_ctx = ctx.shape[1]
    cos, sin = axial_rope_tables(n_lat)
    h_lat, h_ctx = x, ctx
    silu_c = jax.nn.silu(c)
    silu_cc = jax.nn.silu(c_ctx)

    for layer in range(DEPTH):
        last = layer == DEPTH - 1
        lam_init = 0.8 - 0.6 * math.exp(-0.3 * layer)
        mod_l = silu_c @ ada_w[layer] + ada_b[layer]
        mod_c = silu_cc @ ada_w[layer] + ada_b[layer]
        sh1_l, sc1_l, g1_l, sh2_l, sc2_l, g2_l = jnp.split(mod_l[:, None, :], 6, axis=-1)
        sh1_c, sc1_c, g1_c, sh2_c, sc2_c, g2_c = jnp.split(mod_c, 6, axis=-1)
        lv = diff_lambda[layer].astype(jnp.float32)
        lam = jnp.exp(jnp.sum(lv[0] * lv[1])) - jnp.exp(jnp.sum(lv[2] * lv[3])) + lam_init
        w_in_l = w_in[layer]
        branch_params = (w_attn_out[layer], sc_conv_w[layer], w_sc_out[layer], cf_dw_w[layer],
                         cf_dw_b[layer], cf_ln_g[layer], cf_ln_b[layer], w_cf_out[layer],
                         b_gate[layer], w_mix[layer])

        u_lat = modulate(rmsnorm(h_lat, norm1_g[layer]), sh1_l, sc1_l)
        u_ctx = modulate(rmsnorm(h_ctx, norm1_g[layer]), sh1_c, sc1_c)
        p_lat = u_lat @ w_in_l
        if last:
            kv_ctx = u_ctx @ w_in_l[:, OFF_K:OFF_SC]
            k_ctx = split_qk(kv_ctx[..., :QK_W])
            v_ctx = split_v(kv_ctx[..., QK_W:])
        else:
            p_ctx = u_ctx @ w_in_l
            k_ctx = split_qk(p_ctx[..., OFF_K:OFF_V])
            v_ctx = split_v(p_ctx[..., OFF_V:OFF_SC])

        q_lat = apply_axial_rope(split_qk(p_lat[..., OFF_Q:OFF_K]), cos, sin)
        k_lat = apply_axial_rope(split_qk(p_lat[..., OFF_K:OFF_V]), cos, sin)
        v_lat = split_v(p_lat[..., OFF_V:OFF_SC])
        k_all = jnp.concatenate([k_ctx, k_lat], axis=3)
        v_all = jnp.concatenate([v_ctx, v_lat], axis=2)
        a_lat = diff_heads_out(diff_attention(q_lat, k_all, v_all, lam), subln_g[layer], lam_init)
        mix_lat = merge_branches(p_lat, a_lat, *branch_params)
        if not last:
            q_ctx = split_qk(p_ctx[..., OFF_Q:OFF_K])
            a_ctx = diff_heads_out(diff_attention(q_ctx, k_ctx, v_ctx, lam), subln_g[layer], lam_init)
            h_ctx = h_ctx + g1_c * merge_branches(p_ctx, a_ctx, *branch_params)
        h_lat = h_lat + g1_l * mix_lat

        f_lat = modulate(rmsnorm(h_lat, norm2_g[layer]), sh2_l, sc2_l)
        moe_args = (layer, router_g_w[layer], router_g_b[layer], router_e_w[layer],
                    router_e_b[layer], exp_w_gu, exp_w_down)
        if last:
            f = hier_moe(f_lat.reshape(-1, d), *moe_args).reshape(bsz, n_lat, d)
            h_lat = h_lat + g2_l * f
        else:
            f_ctx = modulate(rmsnorm(h_ctx, norm2_g[layer]), sh2_c, sc2_c)
            toks = jnp.concatenate([f_ctx, f_lat], axis=1).reshape(-1, d)
            f = hier_moe(toks, *moe_args).reshape(bsz, n_ctx + n_lat, d)
            h_ctx = h_ctx + g2_c * f[:, :n_ctx]
            h_lat = h_lat + g2_l * f[:, n_ctx:]

    return rmsnorm(h_lat, final_g)
```

```python
import math
from contextlib import ExitStack
import numpy as np
import concourse.bass as bass
import concourse.mybir as mybir
from concourse.bass_utils import run_bass_kernel_spmd

F32 = mybir.dt.float32
BF16 = mybir.dt.bfloat16
I32 = mybir.dt.int32
AF = mybir.ActivationFunctionType
ALU = mybir.AluOpType
AX = mybir.AxisListType

D = 2048
KC = 16
NCTX = 256
NLAT = 2048
T = NCTX + NLAT
NT = T // 128
DEPTH = 2
QK_W = 1024
OFF_Q, OFF_K, OFF_V, OFF_SC, OFF_CF, OFF_GATE = 0, 1024, 2048, 3072, 4608, 5632
C_TOT = 11776
NE = 32
CAP = 512
NST = CAP // 128
EH = 1024
NBATCH = 4
BIG = 1.0e30


class Buf:
    __slots__ = ("w", "r")

    def __init__(self):
        self.w = None
        self.r = {}


def bufs(n):
    return [Buf() for _ in range(n)]


class Eng:
    LIMIT = 30000

    def __init__(self, ctx, name, e):
        self.ctx, self.name, self.e = ctx, name, e
        self.nsem = 0
        self.waited = {}
        self.sem = None
        self.cnt = 0
        self._newsem()

    def _newsem(self):
        self.sem = self.ctx.sem(f"{self.name}_s{self.nsem}")
        self.nsem += 1
        self.cnt = 0

    def wait(self, tok):
        if tok is None:
            return
        s, v = tok
        if self.waited.get(id(s), 0) >= v:
            return
        self.e.wait_ge(s, v)
        self.waited[id(s)] = v

    def deps(self, reads, writes):
        for b in reads:
            self.wait(b.w)
        for b in writes:
            self.wait(b.w)
            for t in list(b.r.values()):
                self.wait(t)

    @staticmethod
    def done(tok, reads, writes):
        for b in reads:
            b.r[id(tok[0])] = tok
        for b in writes:
            b.w = tok
            b.r = {}

    def op(self, fn, reads=(), writes=(), skip_out_deps=False):
        self.deps(reads, () if skip_out_deps else writes)
        if self.cnt >= self.LIMIT:
            self._newsem()
        ins = fn(self.e)
        self.cnt += 1
        ins.then_inc(self.sem, 1)
        tok = (self.sem, self.cnt)
        self.done(tok, reads, writes)
        self.ctx.last[self.name] = tok
        return tok


class DmaQ:
    def __init__(self, ctx, name, eng, npool=8):
        self.ctx, self.eng = ctx, eng
        self.pool = [ctx.sem(f"{name}_d{i}") for i in range(npool)]
        self.vals = [0] * npool
        self.i = 0

    def _issue(self, fn, reads, writes):
        E = self.eng
        E.deps(reads, writes)
        i = self.i
        s = self.pool[i]
        if self.vals[i] > 0:
            E.wait((s, self.vals[i]))
        ins = fn(E.e)
        self.vals[i] += 16
        ins.then_inc(s, 16)
        tok = (s, self.vals[i])
        self.i = (i + 1) % len(self.pool)
        Eng.done(tok, reads, writes)
        return tok

    def dma(self, out, in_, reads=(), writes=(), **kw):
        return self._issue(lambda e: e.dma_start(out=out, in_=in_, **kw), reads, writes)

    def outstanding(self):
        return [(s, v) for s, v in zip(self.pool, self.vals) if v > 0]


class Ctx:
    def __init__(self, nc):
        self.nc = nc
        self.es = ExitStack()
        self.last = {}
        self.nsb = 0
        self.PE = Eng(self, "pe", nc.tensor)
        self.ACT = Eng(self, "act", nc.scalar)
        self.DVE = Eng(self, "dve", nc.vector)
        self.POOL = Eng(self, "pool", nc.gpsimd)
        self.SP = Eng(self, "sp", nc.sync)
        self.spq = DmaQ(self, "spq", self.SP, 12)
        self.plq = DmaQ(self, "plq", self.POOL, 8)
        self.engs = [self.PE, self.ACT, self.DVE, self.POOL, self.SP]

    def sem(self, name):
        return self.es.enter_context(self.nc.semaphore(name))

    def sb(self, shape, dt, stack=None, name=None):
        self.nsb += 1
        st = stack if stack is not None else self.es
        return st.enter_context(self.nc.sbuf_tensor(name or f"sb{self.nsb}", list(shape), dt))

    def barrier(self):
        toks = list(self.last.values()) + self.spq.outstanding() + self.plq.outstanding()
        for E in self.engs:
            for t in toks:
                E.wait(t)


def build(n_layers=DEPTH, with_moe=True, dbg=False):
    nc = bass.Bass("TRN2", target_bir_lowering=False)
    C = Ctx(nc)
    PE, ACT, DVE, POOL, SP = C.PE, C.ACT, C.DVE, C.POOL, C.SP
    spq, plq = C.spq, C.plq

    def din(name, shape, dt=F32):
        return nc.dram_tensor(name, list(shape), dt, kind="ExternalInput").ap()

    xin = din("xin", [T, D])
    cvec = din("cvec", [128, KC, 2])
    ada_w = din("ada_w", [n_layers, 24, 128, KC, 512])
    ada_bT = din("ada_bT", [n_layers, 128, 96])
    n1gT = din("n1gT", [n_layers, 128, KC])
    n2gT = din("n2gT", [n_layers, 128, KC])
    fgT = din("fgT", [128, KC])
    w_in = din("w_in", [n_layers, C_TOT // 128, 128, KC, 128])
    bgT = din("bgT", [n_layers, 128, 48])
    dlam = din("dlam", [n_layers, 256])
    sgT = din("sgT", [n_layers, 128, 1])
    w_o_in = din("w_o", [n_layers, KC, 128, KC, 128])
    w_mix = din("w_mix", [n_layers, KC, 128, KC, 128])
    scwT = din("scwT", [n_layers, 128, 4, 3])
    cfwT = din("cfwT", [n_layers, 128, 4, 31])
    cfbT = din("cfbT", [n_layers, 128, 4])
    lngT = din("lngT", [n_layers, 128, 4])
    lnbT = din("lnbT", [n_layers, 128, 4])
    consts = din("consts", [128, 640 + 2 * NLAT])
    if with_moe:
        wr = din("wr", [n_layers, D, 36])
        rb = din("rb", [n_layers, 1, 36])
        w_gu = din("w_gu", [n_layers, NE, 4, 128, KC, 512])
        w_dn = din("w_dn", [n_layers, NE, 4, 128, 8, 512])
    out = nc.dram_tensor("out", [NLAT, D], F32, kind="ExternalOutput").ap()
    dbg_h = nc.dram_tensor("dbg_h", [D, T], F32, kind="ExternalOutput").ap() if dbg else None
    dbg_br = nc.dram_tensor("dbg_br", [D, T], BF16, kind="ExternalOutput").ap() if dbg else None

    hT = nc.dram_tensor("hT", [D, T], F32, kind="Internal").ap()
    brT = nc.dram_tensor("brT", [D, T], BF16, kind="Internal").ap()
    if with_moe:
        slotbuf = nc.dram_tensor("slotbuf", [NE * CAP, D], BF16, kind="Internal").ap()
        yslot = nc.dram_tensor("yslot", [NE * CAP, D], F32, kind="Internal").ap()
    hT3 = hT.rearrange("(k p) t -> p k t", p=128)
    brT3 = brT.rearrange("(k p) t -> p k t", p=128)
    HTBK = [bufs(NT) for _ in range(KC)]

    def htb(tiles, k=None):
        ks = range(KC) if k is None else [k]
        return [HTBK[k_][i_] for k_ in ks for i_ in tiles]
    BRB = [bufs(NT) for _ in range(KC)]
    SLOTB, YSLOTB = Buf(), Buf()

    cst = C.sb([128, 640], F32, name="cst")
    CST = Buf()
    spq.dma(cst[:], consts[:, 0:640], writes=[CST])
    ident_f = cst[:, 0:128]
    ones_f = cst[:, 128:256]
    rperm_f = cst[:, 256:384]
    tri_f = cst[:, 384:512]
    ecap_f = cst[:, 512:544]
    eps6 = cst[:, 544:545]
    eps5 = cst[:, 545:546]
    cb = C.sb([128, 384], BF16, name="cb")
    CB = Buf()
    DVE.op(lambda e: e.tensor_copy(cb[:, 0:128], ident_f), [CST], [CB])
    DVE.op(lambda e: e.tensor_copy(cb[:, 128:256], ones_f), [CST], [CB])
    DVE.op(lambda e: e.tensor_copy(cb[:, 256:384], tri_f), [CST], [CB])
    ident_b, ones_b, tri_b = cb[:, 0:128], cb[:, 128:256], cb[:, 256:384]

    modT = C.sb([128, n_layers, 96, 2], F32, name="modT")
    A1 = C.sb([128, n_layers, KC, 2], F32, name="A1")
    A2 = C.sb([128, n_layers, KC, 2], F32, name="A2")
    MOD = bufs(n_layers)
    small = C.sb([128, n_layers, 160], F32, name="small")
    cfw = C.sb([128, n_layers, 4 * 31 + 12 + 12], F32, name="cfw")
    fg = C.sb([128, KC], F32, name="fg")
    lamt = C.sb([128, n_layers, 4], F32, name="lamt")
    SMALL = Buf()
    for l in range(n_layers):
        spq.dma(small[:, l, 0:16], n1gT[l], writes=[SMALL])
        spq.dma(small[:, l, 16:32], n2gT[l], writes=[SMALL])
        spq.dma(small[:, l, 32:80], bgT[l], writes=[SMALL])
        spq.dma(small[:, l, 80:81], sgT[l], writes=[SMALL])
        spq.dma(cfw[:, l, 0:124], cfwT[l].rearrange("p c k -> p (c k)"), writes=[SMALL])
        spq.dma(cfw[:, l, 124:128], cfbT[l], writes=[SMALL])
        spq.dma(cfw[:, l, 128:132], lngT[l], writes=[SMALL])
        spq.dma(cfw[:, l, 132:136], lnbT[l], writes=[SMALL])
        spq.dma(cfw[:, l, 136:148], scwT[l].rearrange("p c k -> p (c k)"), writes=[SMALL])
    spq.dma(fg[:], fgT, writes=[SMALL])

    pscnt = [0]

    def mkps(stack, nf=6, nb=2):
        pscnt[0] += 1
        t = pscnt[0]
        ps_f = [stack.enter_context(nc.psum_tensor(f"ps{t}_{i}", [128, 512], F32)) for i in range(nf)]
        ps_b = [stack.enter_context(nc.psum_tensor(f"psb{t}_{i}", [128, 1024], BF16)) for i in range(nb)]
        return ps_f, bufs(nf), ps_b, bufs(nb)

    def mm(out_ap, ob, lhsT, lb, rhs, rb_, start, stop):
        PE.op(lambda e: e.matmul(out_ap, lhsT, rhs, start=start, stop=stop),
              list(lb) + list(rb_), [ob], skip_out_deps=not start)

    def tr(out_ap, ob, in_ap, ib, idn, first):
        PE.op(lambda e: e.transpose(out_ap, in_ap, idn), list(ib) + [CST, CB], [ob], skip_out_deps=not first)

    def load_w(dst3, WB, src3):
        return plq.dma(dst3, src3, writes=[WB])

    def p0_gen(l, ph, pbanks):
        cv = C.sb([128, KC, 2], F32, ph)
        sT = C.sb([128, KC, 2], BF16, ph)
        CV, ST = Buf(), Buf()
        adab = C.sb([128, 96], F32, ph)
        ADAB = Buf()
        wt = [C.sb([128, KC, 512], BF16, ph) for _ in range(2)]
        WT = bufs(2)
        lv = C.sb([128, 256], F32, ph)
        LV = Buf()
        pr = C.sb([128, 2, 64], F32, ph)
        PR = Buf()
        sm = C.sb([128, 4], F32, ph)
        SM = Buf()
        spq.dma(cv[:], cvec, writes=[CV])
        spq.dma(adab[:], ada_bT[l], writes=[ADAB])
        spq.dma(lv[:], dlam[l].partition_broadcast(128), writes=[LV])
        ACT.op(lambda e: e.activation(out=sT[:], in_=cv[:], func=AF.Silu), [CV], [ST])
        for cg in range(24):
            w_, W_ = wt[cg % 2], WT[cg % 2]
            pb, PB_ = pbanks[cg % 2]
            load_w(w_[:], W_, ada_w[l, cg])
            for cc in range(4):
                for k in range(KC):
                    mm(pb[:, cc * 2:cc * 2 + 2], PB_, w_[:, k, cc * 128:(cc + 1) * 128], [W_], sT[:, k, :], [ST], k == 0, k == KC - 1)
            src = pb[:, 0:8].rearrange("p (a b) -> p a b", a=4)
            DVE.op(lambda e: e.tensor_copy(modT[:, l, cg * 4:(cg + 1) * 4, :], src), [PB_], [MOD[l]])
            yield
        for r in range(2):
            DVE.op(lambda e: e.tensor_tensor(out=modT[:, l, :, r], in0=modT[:, l, :, r], in1=adab[:], op=ALU.add), [MOD[l], ADAB], [MOD[l]])
            DVE.op(lambda e: e.scalar_tensor_tensor(out=A1[:, l, :, r], in0=modT[:, l, 16:32, r], scalar=1.0, in1=small[:, l, 0:16], op0=ALU.add, op1=ALU.mult), [MOD[l], SMALL], [MOD[l]])
            DVE.op(lambda e: e.scalar_tensor_tensor(out=A2[:, l, :, r], in0=modT[:, l, 64:80, r], scalar=1.0, in1=small[:, l, 16:32], op0=ALU.add, op1=ALU.mult), [MOD[l], SMALL], [MOD[l]])
        yield
        lam_init = 0.8 - 0.6 * math.exp(-0.3 * l)
        DVE.op(lambda e: e.tensor_tensor(out=pr[:, 0, :], in0=lv[:, 0:64], in1=lv[:, 64:128], op=ALU.mult), [LV], [PR])
        DVE.op(lambda e: e.tensor_tensor(out=pr[:, 1, :], in0=lv[:, 128:192], in1=lv[:, 192:256], op=ALU.mult), [LV], [PR])
        DVE.op(lambda e: e.tensor_reduce(out=sm[:, 0:2], in_=pr[:], axis=AX.X, op=ALU.add), [PR], [SM])
        ACT.op(lambda e: e.activation(out=sm[:, 2:4], in_=sm[:, 0:2], func=AF.Exp), [SM], [SM])
        DVE.op(lambda e: e.scalar_tensor_tensor(out=lamt[:, l, 0:1], in0=sm[:, 3:4], scalar=-lam_init, in1=sm[:, 2:3], op0=ALU.add, op1=ALU.subtract), [SM], [MOD[l]])
        DVE.op(lambda e: e.tensor_scalar(out=lamt[:, l, 1:2], in0=small[:, l, 80:81], scalar1=(1.0 - lam_init), scalar2=None, op0=ALU.mult), [SMALL], [MOD[l]])
        yield

    with ExitStack() as ph:
        psum, PSB, psum_b, PSBB = mkps(ph, 6, 2)
        xb = [C.sb([128, D], F32, ph) for _ in range(2)]
        XB = bufs(2)
        stg = [C.sb([128, KC, 128], F32, ph) for _ in range(2)]
        STG = bufs(2)
        p0gs = [p0_gen(l_, ph, [(psum[4], PSB[4]), (psum[5], PSB[5])]) for l_ in range(n_layers)]

        def p0_step(n_):
            for _ in range(n_):
                while p0gs:
                    try:
                        next(p0gs[0])
                        break
                    except StopIteration:
                        p0gs.pop(0)
        for i in range(NT):
            x_, X_ = xb[i % 2], XB[i % 2]
            s_, S_ = stg[i % 2], STG[i % 2]
            spq.dma(x_[:], xin[i * 128:(i + 1) * 128, :], writes=[X_])
            for g in range(4):
                for kk in range(4):
                    k = g * 4 + kk
                    tr(psum[g][:, kk * 128:(kk + 1) * 128], PSB[g], x_[:, k * 128:(k + 1) * 128], [X_], ident_f, kk == 0)
                E = ACT if g % 2 else DVE
                src = psum[g][:, :].rearrange("p (a b) -> p a b", a=4)
                if E is ACT:
                    E.op(lambda e: e.copy(s_[:, g * 4:(g + 1) * 4, :], src), [PSB[g]], [S_])
                else:
                    E.op(lambda e: e.tensor_copy(s_[:, g * 4:(g + 1) * 4, :], src), [PSB[g]], [S_])
            spq.dma(hT3[:, :, i * 128:(i + 1) * 128], s_[:], reads=[S_], writes=htb([i]))
            p0_step(3)
        p0_step(100)
        C.barrier()


    def col(ap4, l, j, r):
        return ap4[:, l, j, r:r + 1]

    UTB = [bufs(KC) for _ in range(9)]

    def ut_bufs(t0, n):
        cs = range(t0 // 256, (t0 + n + 255) // 256)
        return [UTB[c][k] for c in cs for k in range(KC)]

    def ut_bufs_k(t0, n, k):
        return [UTB[c][k] for c in range(t0 // 256, (t0 + n + 255) // 256)]

    for l in range(n_layers):
        last = l == n_layers - 1 and n_layers == DEPTH
        lastl = l == n_layers - 1
        ctx_full = not (l == DEPTH - 1)
        w_in_l = w_in[l]
        phU = ExitStack()
        uT = C.sb([128, KC, T], BF16, phU, name=f"uT{l}")
        with ExitStack() as ph:
            psum, PSB, psum_b, PSBB = mkps(ph, 6, 2)
            hcb = [C.sb([128, KC, 256], F32, ph) for _ in range(2)]
            HCB = bufs(2)
            sq = C.sb([128, KC, 256], F32, ph)
            SQ = Buf()
            rstd = [C.sb([128, 256], F32, ph) for _ in range(2)]
            RS = bufs(2)
            tmp = [C.sb([128, 256], F32, ph) for _ in range(2)]
            TMP = bufs(2)
            for c in range(9):
                r = 1 if c == 0 else 0
                hc, HC = hcb[c % 2], HCB[c % 2]
                rs_, RS_ = rstd[c % 2], RS[c % 2]
                spq.dma(hc[:], hT3[:, :, c * 256:(c + 1) * 256], reads=htb([2 * c, 2 * c + 1]), writes=[HC])
                ACT.op(lambda e: e.activation(out=sq[:], in_=hc[:], func=AF.Square), [HC], [SQ])
                pb, PB_ = psum[c % 2], PSB[c % 2]
                for k in range(KC):
                    mm(pb[:, 0:256], PB_, ones_f, [CST], sq[:, k, :], [SQ], k == 0, k == KC - 1)
                ACT.op(lambda e: e.activation(out=rs_[:], in_=pb[:, 0:256], func=AF.Sqrt, bias=eps6, scale=1.0 / D), [PB_], [RS_])
                DVE.op(lambda e: e.reciprocal(rs_[:], rs_[:]), [RS_], [RS_])
                for k in range(KC):
                    t_, T_ = tmp[k % 2], TMP[k % 2]
                    DVE.op(lambda e: e.scalar_tensor_tensor(out=t_[:], in0=hc[:, k, :], scalar=col(A1, l, k, r), in1=rs_[:], op0=ALU.mult, op1=ALU.mult), [HC, RS_, MOD[l]], [T_])
                    ACT.op(lambda e: e.activation(out=uT[:, k, c * 256:(c + 1) * 256], in_=t_[:], func=AF.Identity, bias=modT[:, l, 0 + k, r:r + 1], scale=1.0), [T_, MOD[l]], [UTB[c][k]])
            C.barrier()

        with ExitStack() as ph:
            psum, PSB, psum_b, PSBB = mkps(ph, 8, 0)
            rope = C.sb([128, 2 * NLAT], F32, ph)
            ROPE = Buf()
            spq.dma(rope[:], consts[:, 640:640 + 2 * NLAT], writes=[ROPE])
            cosT = rope[:, 0:NLAT]
            sinT = rope[:, NLAT:2 * NLAT]
            wq = [C.sb([128, KC, 128], BF16, ph) for _ in range(2)]
            wk = [C.sb([128, KC, 128], BF16, ph) for _ in range(2)]
            wv = [C.sb([128, KC, 128], BF16, ph) for _ in range(2)]
            WQ, WK, WV = bufs(2), bufs(2), bufs(2)
            QT = [C.sb([128, T], BF16, ph) for _ in range(2)]
            KT = [C.sb([128, T], BF16, ph) for _ in range(2)]
            VH = [C.sb([128, NT, 128], BF16, ph) for _ in range(2)]
            QTB = [bufs(5) for _ in range(2)]
            KTB = [bufs(5) for _ in range(2)]
            VHB = [bufs(NT) for _ in range(2)]
            qf = [C.sb([128, 512], F32, ph) for _ in range(2)]
            QF = bufs(2)
            t1 = [C.sb([128, 512], F32, ph) for _ in range(2)]
            T1 = bufs(2)
            t2 = [C.sb([128, 512], F32, ph) for _ in range(2)]
            T2 = bufs(2)
            ET = [C.sb([128, 512], BF16, ph) for _ in range(6)]
            ETB = bufs(6)
            am = 0
            rd = C.sb([128, 512], F32, ph)
            RD = Buf()
            om = [C.sb([128, 512], F32, ph) for _ in range(2)]
            OM = bufs(2)
            oo = C.sb([128, 512], F32, ph)
            OO = Buf()
            sqo = C.sb([128, 512], F32, ph)
            SQO = Buf()
            rs2 = C.sb([128, 512], F32, ph)
            RS2 = Buf()
            aTh = [C.sb([128, T], BF16, ph) for _ in range(2)]
            ATH = [bufs(5) for _ in range(2)]
            chunks = [(0, 256, True)] + [(256 + 512 * i, 512, False) for i in range(4)]

            def chunk_idx(t0):
                return 0 if t0 == 0 else 1 + (t0 - 256) // 512

            for h in range(8):
                hp = h % 2
                load_w(wq[hp][:], WQ[hp], w_in_l[OFF_Q // 128 + h])
                load_w(wk[hp][:], WK[hp], w_in_l[OFF_K // 128 + h])
                load_w(wv[hp][:], WV[hp], w_in_l[OFF_V // 128 + h])
                pi = 0
                for (w_, W_, dst, DSTB, need_ctx) in ((wq[hp], WQ[hp], QT[hp], QTB[hp], ctx_full), (wk[hp], WK[hp], KT[hp], KTB[hp], True)):
                    for (t0, n, isctx) in chunks:
                        if isctx and not need_ctx:
                            continue
                        ci = chunk_idx(t0)
                        pb, PB_ = psum[3], PSB[3]
                        for k in range(KC):
                            mm(pb[:, 0:n], PB_, w_[:, k, :], [W_], uT[:, k, t0:t0 + n], ut_bufs_k(t0, n, k), k == 0, k == KC - 1)
                        if isctx:
                            ACT.op(lambda e: e.copy(dst[:, t0:t0 + n], pb[:, 0:n]), [PB_], [DSTB[ci]])
                        else:
                            q_, Q_ = qf[pi % 2], QF[pi % 2]
                            a_, A_ = t1[pi % 2], T1[pi % 2]
                            b_, B_ = t2[pi % 2], T2[pi % 2]
                            pi += 1
                            l0 = t0 - NCTX
                            ACT.op(lambda e: e.copy(q_[:], pb[:, 0:n]), [PB_], [Q_])
                            mm(psum[4][:, 0:n], PSB[4], rperm_f, [CST], q_[:], [Q_], True, True)
                            POOL.op(lambda e: e.tensor_tensor(out=a_[:], in0=q_[:], in1=cosT[:, l0:l0 + n], op=ALU.mult), [Q_, ROPE], [A_])
                            DVE.op(lambda e: e.tensor_tensor(out=b_[:], in0=psum[4][:, 0:n], in1=sinT[:, l0:l0 + n], op=ALU.mult), [PSB[4], ROPE], [B_])
                            DVE.op(lambda e: e.tensor_tensor(out=dst[:, t0:t0 + n], in0=a_[:], in1=b_[:], op=ALU.add), [A_, B_], [DSTB[ci]])
                for g in range((NT + 3) // 4):
                    tiles = list(range(g * 4, min(NT, g * 4 + 4)))
                    pb, PB_ = psum[3], PSB[3]
                    for ii, i in enumerate(tiles):
                        for k in range(KC):
                            mm(pb[:, ii * 128:(ii + 1) * 128], PB_, uT[:, k, i * 128:(i + 1) * 128], ut_bufs_k(i * 128, 128, k), wv[hp][:, k, :], [WV[hp]], k == 0, k == KC - 1)
                    nt_ = len(tiles)
                    src = pb[:, 0:nt_ * 128].rearrange("p (a b) -> p a b", a=nt_)
                    ACT.op(lambda e: e.copy(VH[hp][:, g * 4:g * 4 + nt_, :], src), [PB_], [VHB[hp][i] for i in tiles])
                for (q0, n, isctx) in chunks:
                    if isctx and not ctx_full:
                        continue
                    ci = chunk_idx(q0)
                    kts = [0, 1] if isctx else list(range(NT))
                    for m in range(2):
                        pr_ = slice(m * 64, (m + 1) * 64)
                        pO, PO_ = psum[4 + am % 2], PSB[4 + am % 2]
                        pD, PD_ = psum[6 + am % 2], PSB[6 + am % 2]
                        am += 1

                        def S(j):
                            kt = kts[j]
                            kci = chunk_idx(0 if kt < 2 else 256 + ((kt - 2) // 4) * 512)
                            mm(psum[j % 4][:, 0:n], PSB[j % 4], KT[hp][pr_, kt * 128:(kt + 1) * 128], [KTB[hp][kci]], QT[hp][pr_, q0:q0 + n], [QTB[hp][ci]], True, True)
                            ACT.op(lambda e: e.activation(out=ET[j % 6][:, 0:n], in_=psum[j % 4][:, 0:n], func=AF.Exp, scale=0.125), [PSB[j % 4]], [ETB[j % 6]])

                        def OD(j):
                            kt = kts[j]
                            mm(pO[:, 0:n], PO_, VH[hp][:, kt, :], [VHB[hp][kt]], ET[j % 6][:, 0:n], [ETB[j % 6]], j == 0, j == len(kts) - 1)
                            mm(pD[:, 0:n], PD_, ones_b, [CB], ET[j % 6][:, 0:n], [ETB[j % 6]], j == 0, j == len(kts) - 1)

                        for j in range(min(3, len(kts))):
                            S(j)
                        for j in range(len(kts)):
                            if j + 3 < len(kts):
                                S(j + 3)
                            OD(j)
                        DVE.op(lambda e: e.reciprocal(rd[:, 0:n], pD[:, 0:n]), [PD_], [RD])
                        DVE.op(lambda e: e.tensor_tensor(out=om[m][:, 0:n], in0=pO[:, 0:n], in1=rd[:, 0:n], op=ALU.mult), [PO_, RD], [OM[m]])
                    DVE.op(lambda e: e.scalar_tensor_tensor(out=oo[:, 0:n], in0=om[1][:, 0:n], scalar=lamt[:, l, 0:1], in1=om[0][:, 0:n], op0=ALU.mult, op1=ALU.add), [OM[0], OM[1], MOD[l]], [OO])
                    ACT.op(lambda e: e.activation(out=sqo[:, 0:n], in_=oo[:, 0:n], func=AF.Square), [OO], [SQO])
                    mm(psum[5][:, 0:n], PSB[5], ones_f, [CST], sqo[:, 0:n], [SQO], True, True)
                    ACT.op(lambda e: e.activation(out=rs2[:, 0:n], in_=psum[5][:, 0:n], func=AF.Sqrt, bias=eps5, scale=1.0 / 128), [PSB[5]], [RS2])
                    DVE.op(lambda e: e.reciprocal(rs2[:, 0:n], rs2[:, 0:n]), [RS2], [RS2])
                    DVE.op(lambda e: e.scalar_tensor_tensor(out=aTh[hp][:, q0:q0 + n], in0=oo[:, 0:n], scalar=lamt[:, l, 1:2], in1=rs2[:, 0:n], op0=ALU.mult, op1=ALU.mult), [OO, RS2, MOD[l]], [ATH[hp][ci]])
                    tl = list(range(q0 // 128, (q0 + n) // 128))
                    spq.dma(brT[h * 128:(h + 1) * 128, q0:q0 + n], aTh[hp][:, q0:q0 + n], reads=[ATH[hp][ci]], writes=[BRB[h][i] for i in tl])
            C.barrier()

        with ExitStack() as ph:
            psum, PSB, psum_b, PSBB = mkps(ph, 6, 2)
            wa = [C.sb([128, KC, 128], BF16, ph) for _ in range(6)]
            WA = bufs(6)
            seqs = [(NCTX, NLAT)] + ([(0, NCTX)] if ctx_full else [])
            zb = C.sb([128, 4, NLAT], F32, ph)
            ZB = bufs(4)
            cfin = C.sb([128, NLAT + 30], F32, ph)
            CFIN = Buf()
            sg = [C.sb([128, 512], F32, ph) for _ in range(2)]
            SG = bufs(2)
            sqz = C.sb([128, 4, 512], F32, ph)
            SQZ = Buf()
            mean = C.sb([128, 512], F32, ph)
            MEAN = Buf()
            msq = C.sb([128, 512], F32, ph)
            MSQ = Buf()
            var = C.sb([128, 512], F32, ph)
            VAR = Buf()
            zt = [C.sb([128, 512], F32, ph) for _ in range(2)]
            ZT = bufs(2)
            cto = [C.sb([128, 512], BF16, ph) for _ in range(2)]
            CTO = bufs(2)
            bsave = zb[:, 0, :]
            BSAVE = ZB[0]
            ysc = zb[:, 1, :]
            YSC = ZB[1]
            bto = C.sb([128, NLAT], BF16, ph)
            BTO = Buf()
            wi = 0
            for (s0, L) in seqs:
                nch = [(o, min(512, L - o)) for o in range(0, L, 512)]
                for c in range(4):
                    wa_, WA_ = wa[wi % 6], WA[wi % 6]
                    wg_, WG_ = wa[(wi + 1) % 6], WA[(wi + 1) % 6]
                    wi += 2
                    load_w(wa_[:], WA_, w_in_l[OFF_CF // 128 + c])
                    load_w(wg_[:], WG_, w_in_l[OFF_CF // 128 + 4 + c])
                    POOL.op(lambda e: e.memset(cfin[:, 0:15], 0.0), [], [CFIN])
                    POOL.op(lambda e: e.memset(cfin[:, 15 + L:30 + L], 0.0), [], [CFIN])
                    for ii, (o, n) in enumerate(nch):
                        t0 = s0 + o
                        pa, PA_ = psum[0 + 2 * (ii % 2)], PSB[0 + 2 * (ii % 2)]
                        pg, PG_ = psum[1 + 2 * (ii % 2)], PSB[1 + 2 * (ii % 2)]
                        for k in range(KC):
                            mm(pa[:, 0:n], PA_, wa_[:, k, :], [WA_], uT[:, k, t0:t0 + n], ut_bufs_k(t0, n, k), k == 0, k == KC - 1)
                        for k in range(KC):
                            mm(pg[:, 0:n], PG_, wg_[:, k, :], [WG_], uT[:, k, t0:t0 + n], ut_bufs_k(t0, n, k), k == 0, k == KC - 1)
                        s_, S_ = sg[ii % 2], SG[ii % 2]
                        ACT.op(lambda e: e.activation(out=s_[:, 0:n], in_=pg[:, 0:n], func=AF.Sigmoid), [PG_], [S_])
                        DVE.op(lambda e: e.tensor_tensor(out=cfin[:, 15 + o:15 + o + n], in0=pa[:, 0:n], in1=s_[:, 0:n], op=ALU.mult), [PA_, S_], [CFIN])
                    wb0 = c * 31
                    DVE.op(lambda e: e.tensor_scalar(out=zb[:, c, 0:L], in0=cfin[:, 0:L], scalar1=cfw[:, l, wb0:wb0 + 1], scalar2=cfw[:, l, 124 + c:125 + c], op0=ALU.mult, op1=ALU.add), [CFIN, SMALL], [ZB[c]])
                    for kk in range(1, 31):
                        E = DVE
                        E.op(lambda e: e.scalar_tensor_tensor(out=zb[:, c, 0:L], in0=cfin[:, kk:kk + L], scalar=cfw[:, l, wb0 + kk:wb0 + kk + 1], in1=zb[:, c, 0:L], op0=ALU.mult, op1=ALU.add), [CFIN, SMALL], [ZB[c]])
                for ii, (o, n) in enumerate(nch):
                    t0 = s0 + o
                    ACT.op(lambda e: e.activation(out=sqz[:, :, 0:n], in_=zb[:, :, o:o + n], func=AF.Square), ZB, [SQZ])
                    for c in range(4):
                        mm(psum[0][:, 0:n], PSB[0], ones_f, [CST], zb[:, c, o:o + n], [ZB[c]], c == 0, c == 3)
                    for c in range(4):
                        mm(psum[1][:, 0:n], PSB[1], ones_f, [CST], sqz[:, c, 0:n], [SQZ], c == 0, c == 3)
                    ACT.op(lambda e: e.activation(out=mean[:, 0:n], in_=psum[0][:, 0:n], func=AF.Identity, scale=1.0 / 512), [PSB[0]], [MEAN])
                    DVE.op(lambda e: e.tensor_tensor(out=msq[:, 0:n], in0=mean[:, 0:n], in1=mean[:, 0:n], op=ALU.mult), [MEAN], [MSQ])
                    DVE.op(lambda e: e.scalar_tensor_tensor(out=var[:, 0:n], in0=psum[1][:, 0:n], scalar=1.0 / 512, in1=msq[:, 0:n], op0=ALU.mult, op1=ALU.subtract), [PSB[1], MSQ], [VAR])
                    ACT.op(lambda e: e.activation(out=var[:, 0:n], in_=var[:, 0:n], func=AF.Sqrt, bias=eps5, scale=1.0), [VAR], [VAR])
                    DVE.op(lambda e: e.reciprocal(var[:, 0:n], var[:, 0:n]), [VAR], [VAR])
                    for c in range(4):
                        z_, Z_ = zt[c % 2], ZT[c % 2]
                        o_, O_ = cto[c % 2], CTO[c % 2]
                        DVE.op(lambda e: e.tensor_tensor(out=z_[:, 0:n], in0=zb[:, c, o:o + n], in1=mean[:, 0:n], op=ALU.subtract), [ZB[c], MEAN], [Z_])
                        DVE.op(lambda e: e.tensor_tensor(out=z_[:, 0:n], in0=z_[:, 0:n], in1=var[:, 0:n], op=ALU.mult), [Z_, VAR], [Z_])
                        ACT.op(lambda e: e.activation(out=o_[:, 0:n], in_=z_[:, 0:n], func=AF.Silu, bias=cfw[:, l, 132 + c:133 + c], scale=cfw[:, l, 128 + c:129 + c]), [Z_, SMALL], [O_])
                        tl = list(range(t0 // 128, (t0 + n) // 128))
                        spq.dma(brT[1536 + c * 128:1536 + (c + 1) * 128, t0:t0 + n], o_[:, 0:n], reads=[O_], writes=[BRB[12 + c][i] for i in tl])
                for c in range(4):
                    ws = []
                    for part in range(3):
                        w_, W_ = wa[wi % 6], WA[wi % 6]
                        wi += 1
                        load_w(w_[:], W_, w_in_l[OFF_SC // 128 + part * 4 + c])
                        ws.append((w_, W_))
                    POOL.op(lambda e: e.memset(cfin[:, 0:1], 0.0), [], [CFIN])
                    POOL.op(lambda e: e.memset(cfin[:, 1 + L:2 + L], 0.0), [], [CFIN])
                    for ii, (o, n) in enumerate(nch):
                        t0 = s0 + o
                        pbs = [(psum[3 * (ii % 2) + p_], PSB[3 * (ii % 2) + p_]) for p_ in range(3)]
                        for p_ in range(3):
                            for k in range(KC):
                                mm(pbs[p_][0][:, 0:n], pbs[p_][1], ws[p_][0][:, k, :], [ws[p_][1]], uT[:, k, t0:t0 + n], ut_bufs_k(t0, n, k), k == 0, k == KC - 1)
                        s_, S_ = sg[ii % 2], SG[ii % 2]
                        ACT.op(lambda e: e.copy(bsave[:, o:o + n], pbs[0][0][:, 0:n]), [pbs[0][1]], [BSAVE])
                        ACT.op(lambda e: e.copy(s_[:, 0:n], pbs[2][0][:, 0:n]), [pbs[2][1]], [S_])
                        DVE.op(lambda e: e.tensor_tensor(out=cfin[:, 1 + o:1 + o + n], in0=pbs[1][0][:, 0:n], in1=s_[:, 0:n], op=ALU.mult), [pbs[1][1], S_], [CFIN])
                    w0 = 136 + c * 3
                    DVE.op(lambda e: e.tensor_scalar(out=ysc[:, 0:L], in0=cfin[:, 0:L], scalar1=cfw[:, l, w0:w0 + 1], scalar2=None, op0=ALU.mult), [CFIN, SMALL], [YSC])
                    for kk in (1, 2):
                        DVE.op(lambda e: e.scalar_tensor_tensor(out=ysc[:, 0:L], in0=cfin[:, kk:kk + L], scalar=cfw[:, l, w0 + kk:w0 + kk + 1], in1=ysc[:, 0:L], op0=ALU.mult, op1=ALU.add), [CFIN, SMALL, YSC], [YSC])
                    DVE.op(lambda e: e.tensor_tensor(out=bto[:, 0:L], in0=ysc[:, 0:L], in1=bsave[:, 0:L], op=ALU.mult), [YSC, BSAVE], [BTO])
                    tl = list(range(s0 // 128, (s0 + L) // 128))
                    spq.dma(brT[1024 + c * 128:1024 + (c + 1) * 128, s0:s0 + L], bto[:, 0:L], reads=[BTO], writes=[BRB[8 + c][i] for i in tl])
            C.barrier()

        if dbg and l == 0:
            spq.dma(dbg_br, brT, reads=[b for bb in BRB for b in bb])

        with ExitStack() as ph:
            psum, PSB, psum_b, PSBB = mkps(ph, 6, 2)
            brc = [C.sb([128, KC, 512], BF16, ph) for _ in range(2)]
            BRC = bufs(2)
            NSL = 8
            slab = [C.sb([128, KC, 128], BF16, ph) for _ in range(NSL)]
            SLAB = bufs(NSL)
            sl_i = [0]

            def next_slab(src3):
                i_ = sl_i[0] % NSL
                sl_i[0] += 1
                load_w(slab[i_][:], SLAB[i_], src3)
                return slab[i_], SLAB[i_]
            gt = [C.sb([128, 512], F32, ph) for _ in range(3)]
            GT = bufs(3)
            ma = C.sb([128, 512], F32, ph)
            MA = Buf()
            mb = C.sb([128, 512], F32, ph)
            MB = Buf()
            mT = [C.sb([128, KC, 512], BF16, ph) for _ in range(2)]
            MT = [bufs(KC) for _ in range(2)]
            hold = [C.sb([128, 512], F32, ph) for _ in range(2)]
            HOLD = bufs(2)
            hnew = [C.sb([128, 512], F32, ph) for _ in range(2)]
            HNEW = bufs(2)
            supers = [[(256 + 1024 * sc_ + 512 * ss, 512, 0) for ss in range(2)] for sc_ in range(2)]
            if ctx_full:
                supers.append([(0, 256, 1)])
            hi = 0
            for subs in supers:
                for si, (t0, n, r) in enumerate(subs):
                    tl = list(range(t0 // 128, (t0 + n) // 128))
                    spq.dma(brc[si][:, :, 0:n], brT3[:, :, t0:t0 + n], reads=[BRB[k][i] for k in range(KC) for i in tl], writes=[BRC[si]])
                for j in range(KC):
                    gsl = [next_slab(w_in_l[OFF_GATE // 128 + br * 16 + j]) for br in range(3)]
                    w_o, W_O = next_slab(w_o_in[l, j])
                    for si, (t0, n, r) in enumerate(subs):
                        b_, B_ = brc[si], BRC[si]
                        for br in range(3):
                            for k in range(KC):
                                mm(psum[br][:, 0:n], PSB[br], gsl[br][0][:, k, :], [gsl[br][1]], uT[:, k, t0:t0 + n], ut_bufs_k(t0, n, k), k == 0, k == KC - 1)
                            ACT.op(lambda e: e.activation(out=gt[br][:, 0:n], in_=psum[br][:, 0:n], func=AF.Sigmoid, bias=small[:, l, 32 + br * 16 + j:33 + br * 16 + j], scale=1.0), [PSB[br], SMALL], [GT[br]])
                        for br, (k0, k1) in enumerate(((0, 8), (8, 12), (12, 16))):
                            for k in range(k0, k1):
                                mm(psum[3 + br][:, 0:n], PSB[3 + br], w_o[:, k, :], [W_O], b_[:, k, 0:n], [B_], k == k0, k == k1 - 1)
                        DVE.op(lambda e: e.tensor_tensor(out=ma[:, 0:n], in0=psum[3][:, 0:n], in1=gt[0][:, 0:n], op=ALU.mult), [PSB[3], GT[0]], [MA])
                        DVE.op(lambda e: e.tensor_tensor(out=mb[:, 0:n], in0=psum[4][:, 0:n], in1=gt[1][:, 0:n], op=ALU.mult), [PSB[4], GT[1]], [MB])
                        DVE.op(lambda e: e.tensor_tensor(out=ma[:, 0:n], in0=ma[:, 0:n], in1=mb[:, 0:n], op=ALU.add), [MA, MB], [MA])
                        DVE.op(lambda e: e.tensor_tensor(out=mb[:, 0:n], in0=psum[5][:, 0:n], in1=gt[2][:, 0:n], op=ALU.mult), [PSB[5], GT[2]], [MB])
                        DVE.op(lambda e: e.tensor_tensor(out=mT[si][:, j, 0:n], in0=ma[:, 0:n], in1=mb[:, 0:n], op=ALU.add), [MA, MB], [MT[si][j]])
                for j2 in range(KC):
                    w_m, W_M = next_slab(w_mix[l, j2])
                    for si, (t0, n, r) in enumerate(subs):
                        tl = list(range(t0 // 128, (t0 + n) // 128))
                        ho, HO = hold[hi % 2], HOLD[hi % 2]
                        hn, HN = hnew[hi % 2], HNEW[hi % 2]
                        pb, PB_ = psum[hi % 2], PSB[hi % 2]
                        hi += 1
                        spq.dma(ho[:, 0:n], hT[j2 * 128:(j2 + 1) * 128, t0:t0 + n], reads=htb(tl, j2), writes=[HO])
                        for k in range(KC):
                            mm(pb[:, 0:n], PB_, w_m[:, k, :], [W_M], mT[si][:, k, 0:n], [MT[si][k]], k == 0, k == KC - 1)
                        DVE.op(lambda e: e.scalar_tensor_tensor(out=hn[:, 0:n], in0=pb[:, 0:n], scalar=modT[:, l, 32 + j2, r:r + 1], in1=ho[:, 0:n], op0=ALU.mult, op1=ALU.add), [PB_, HO, MOD[l]], [HN])
                        spq.dma(hT[j2 * 128:(j2 + 1) * 128, t0:t0 + n], hn[:, 0:n], reads=[HN], writes=htb(tl, j2))
            C.barrier()

        phU.close()
        if dbg and l == 0 and not with_moe:
            spq.dma(dbg_h, hT, reads=htb(range(NT)))

        if not with_moe:
            continue

        tiles4 = list(range(NT)) if ctx_full else list(range(2, NT))
        with ExitStack() as ph4:
            sid = C.sb([128, NT, 2], I32, ph4)
            wts = C.sb([128, NT, 2], F32, ph4)
            SIDB = bufs(NT)
            with ExitStack() as ph:
                psum, PSB, psum_b, PSBB = mkps(ph, 8, 0)
                p0g = None
                hcb = [C.sb([128, KC, 256], F32, ph) for _ in range(2)]
                HCB = bufs(2)
                sq = C.sb([128, KC, 256], F32, ph)
                SQ = Buf()
                rstd = [C.sb([128, 256], F32, ph) for _ in range(2)]
                RS = bufs(2)
                tmp = [C.sb([128, 256], F32, ph) for _ in range(2)]
                TMP = bufs(2)
                fT = [C.sb([128, KC, 256], F32, ph) for _ in range(2)]
                FT = bufs(2)
                wrs = C.sb([128, KC, 36], F32, ph)
                rbs = C.sb([1, 36], F32, ph)
                WRS = Buf()
                spq.dma(wrs[:], wr[l].rearrange("(k p) c -> p k c", p=128), writes=[WRS])
                spq.dma(rbs[:], rb[l], writes=[WRS])
                tot = C.sb([128, 32], F32, ph)
                TOT = Buf()
                DVE.op(lambda e: e.memset(tot[:], 0.0), [], [TOT])
                ftok = [C.sb([128, D], BF16, ph) for _ in range(2)]
                FTOK = bufs(2)
                sm_ = [C.sb([128, 512], F32, ph) for _ in range(2)]
                SMB = bufs(2)
                ohb = [C.sb([128, 32], BF16, ph) for _ in range(2)]
                OHB = bufs(2)
                chunks4 = list(range(9)) if ctx_full else list(range(1, 9))
                ti = 0
                for c in chunks4:
                    r = 1 if c == 0 else 0
                    hc, HC = hcb[c % 2], HCB[c % 2]
                    rs_, RS_ = rstd[c % 2], RS[c % 2]
                    f_, F_ = fT[c % 2], FT[c % 2]
                    spq.dma(hc[:], hT3[:, :, c * 256:(c + 1) * 256], reads=htb([2 * c, 2 * c + 1]), writes=[HC])
                    ACT.op(lambda e: e.activation(out=sq[:], in_=hc[:], func=AF.Square), [HC], [SQ])
                    pb, PB_ = psum[4], PSB[4]
                    for k in range(KC):
                        mm(pb[:, 0:256], PB_, ones_f, [CST], sq[:, k, :], [SQ], k == 0, k == KC - 1)
                    ACT.op(lambda e: e.activation(out=rs_[:], in_=pb[:, 0:256], func=AF.Sqrt, bias=eps6, scale=1.0 / D), [PB_], [RS_])
                    DVE.op(lambda e: e.reciprocal(rs_[:], rs_[:]), [RS_], [RS_])
                    for k in range(KC):
                        t_, T_ = tmp[k % 2], TMP[k % 2]
                        DVE.op(lambda e: e.scalar_tensor_tensor(out=t_[:], in0=hc[:, k, :], scalar=col(A2, l, k, r), in1=rs_[:], op0=ALU.mult, op1=ALU.mult), [HC, RS_, MOD[l]], [T_])
                        ACT.op(lambda e: e.activation(out=f_[:, k, :], in_=t_[:], func=AF.Identity, bias=modT[:, l, 48 + k, r:r + 1], scale=1.0), [T_, MOD[l]], [F_])
                    def tile_gen(half, ti, f_=f_, F_=F_, c=c):
                        i = 2 * c + half
                        cs = slice(half * 128, (half + 1) * 128)
                        s_, S_ = sm_[ti % 2], SMB[ti % 2]
                        oh_, OH_ = ohb[ti % 2], OHB[ti % 2]
                        fk, FK = ftok[ti % 2], FTOK[ti % 2]
                        pl, PL_ = psum[5 - half], PSB[5 - half]
                        for k in range(KC):
                            mm(pl[:, 0:36], PL_, f_[:, k, cs], [F_], wrs[:, k, :], [WRS], k == 0, False)
                        mm(pl[:, 0:36], PL_, ones_f[0:1, :], [CST], rbs[0:1, :], [WRS], False, True)
                        lg = s_[:, 0:36]
                        gmax, ngmax, sum4, pg = s_[:, 36:37], s_[:, 37:38], s_[:, 38:39], s_[:, 39:40]
                        gmask, pen, ex4 = s_[:, 40:44], s_[:, 44:48], s_[:, 48:52]
                        lem = s_[:, 64:96]
                        oh1, oh2, lem2 = s_[:, 96:128], s_[:, 128:160], s_[:, 160:192]
                        m1v, m2v, dd, e2, rden = s_[:, 192:193], s_[:, 193:194], s_[:, 194:195], s_[:, 195:196], s_[:, 196:197]
                        rk, tm, oh = s_[:, 200:232], s_[:, 232:264], s_[:, 264:296]
                        sf = s_[:, 296:298]
                        V = lambda fn, rd_=(), wr_=(): DVE.op(fn, [S_] + list(rd_), [S_] + list(wr_))
                        DVE.op(lambda e: e.tensor_copy(lg, pl[:, 0:36]), [PL_], [S_])
                        yield
                        V(lambda e: e.tensor_reduce(out=gmax, in_=s_[:, 0:4], axis=AX.X, op=ALU.max))
                        yield
                        V(lambda e: e.tensor_scalar(out=gmask, in0=s_[:, 0:4], scalar1=gmax, scalar2=None, op0=ALU.is_equal))
                        yield
                        V(lambda e: e.tensor_scalar(out=ngmax, in0=gmax, scalar1=-1.0, scalar2=None, op0=ALU.mult))
                        yield
                        ACT.op(lambda e: e.activation(out=ex4, in_=s_[:, 0:4], func=AF.Exp, bias=ngmax, scale=1.0), [S_], [S_])
                        yield
                        V(lambda e: e.tensor_reduce(out=sum4, in_=ex4, axis=AX.X, op=ALU.add))
                        yield
                        V(lambda e: e.reciprocal(pg, sum4))
                        yield
                        V(lambda e: e.tensor_scalar(out=pen, in0=gmask, scalar1=-1.0, scalar2=BIG, op0=ALU.add, op1=ALU.mult))
                        yield
                        for g in range(4):
                            V(lambda e: e.tensor_scalar(out=s_[:, 64 + g * 8:72 + g * 8], in0=s_[:, 4 + g * 8:12 + g * 8], scalar1=pen[:, g:g + 1], scalar2=None, op0=ALU.add))
                        V(lambda e: e.tensor_reduce(out=m1v, in_=lem, axis=AX.X, op=ALU.max))
                        yield
                        V(lambda e: e.tensor_scalar(out=oh1, in0=lem, scalar1=m1v, scalar2=None, op0=ALU.is_equal))
                        yield
                        V(lambda e: e.scalar_tensor_tensor(out=lem2, in0=oh1, scalar=-BIG, in1=lem, op0=ALU.mult, op1=ALU.add))
                        yield
                        V(lambda e: e.tensor_reduce(out=m2v, in_=lem2, axis=AX.X, op=ALU.max))
                        yield
                        V(lambda e: e.tensor_scalar(out=oh2, in0=lem2, scalar1=m2v, scalar2=None, op0=ALU.is_equal))
                        yield
                        V(lambda e: e.tensor_tensor(out=dd, in0=m2v, in1=m1v, op=ALU.subtract))
                        yield
                        ACT.op(lambda e: e.activation(out=e2, in_=dd, func=AF.Exp), [S_], [S_])
                        yield
                        V(lambda e: e.tensor_scalar(out=rden, in0=e2, scalar1=1.0, scalar2=None, op0=ALU.add))
                        yield
                        V(lambda e: e.reciprocal(rden, rden))
                        yield
                        V(lambda e: e.tensor_tensor(out=wts[:, i, 0:1], in0=pg, in1=rden, op=ALU.mult), [], [SIDB[i]])
                        yield
                        V(lambda e: e.tensor_tensor(out=wts[:, i, 1:2], in0=wts[:, i, 0:1], in1=e2, op=ALU.mult), [SIDB[i]], [SIDB[i]])
                        yield
                        V(lambda e: e.tensor_tensor(out=oh, in0=oh1, in1=oh2, op=ALU.add))
                        yield
                        DVE.op(lambda e: e.tensor_copy(oh_[:], oh), [S_], [OH_])
                        yield
                        pr_, PR_ = psum[3], PSB[3]
                        ro = half * 64
                        mm(pr_[:, ro:ro + 32], PR_, tri_b, [CB], oh_[:], [OH_], True, True)
                        mm(pr_[:, ro + 32:ro + 64], PR_, ones_b, [CB], oh_[:], [OH_], True, True)
                        DVE.op(lambda e: e.tensor_tensor(out=rk, in0=pr_[:, ro:ro + 32], in1=tot[:], op=ALU.add), [PR_, TOT, S_], [S_])
                        yield
                        V(lambda e: e.tensor_scalar(out=rk, in0=rk, scalar1=float(CAP - 1), scalar2=None, op0=ALU.min))
                        yield
                        V(lambda e: e.tensor_tensor(out=rk, in0=rk, in1=ecap_f, op=ALU.add), [CST])
                        yield
                        DVE.op(lambda e: e.tensor_tensor(out=tot[:], in0=tot[:], in1=pr_[:, ro + 32:ro + 64], op=ALU.add), [PR_, TOT], [TOT])
                        yield
                        V(lambda e: e.tensor_tensor(out=tm, in0=rk, in1=oh1, op=ALU.mult))
                        yield
                        V(lambda e: e.tensor_reduce(out=sf[:, 0:1], in_=tm, axis=AX.X, op=ALU.add))
                        yield
                        V(lambda e: e.tensor_tensor(out=tm, in0=rk, in1=oh2, op=ALU.mult))
                        yield
                        V(lambda e: e.tensor_reduce(out=sf[:, 1:2], in_=tm, axis=AX.X, op=ALU.add))
                        yield
                        V(lambda e: e.tensor_copy(sid[:, i, :], sf), [], [SIDB[i]])
                        yield
                        for g in range(4):
                            for kk in range(4):
                                k = g * 4 + kk
                                tr(psum[g % 3][:, kk * 128:(kk + 1) * 128], PSB[g % 3], f_[:, k, cs], [F_], ident_f, kk == 0)
                            E = ACT if g % 2 else DVE
                            if E is ACT:
                                E.op(lambda e: e.copy(fk[:, g * 512:(g + 1) * 512], psum[g % 3][:, :]), [PSB[g % 3]], [FK])
                            else:
                                E.op(lambda e: e.tensor_copy(fk[:, g * 512:(g + 1) * 512], psum[g % 3][:, :]), [PSB[g % 3]], [FK])
                        for kk in range(2):
                            plq._issue(lambda e: e.indirect_dma_start(out=slotbuf[:, :], out_offset=bass.IndirectOffsetOnAxis(ap=sid[:, i, kk:kk + 1], axis=0), in_=fk[:, :], in_offset=None), [FK, SIDB[i]], [SLOTB])
                    gens = [tile_gen(0, ti), tile_gen(1, ti + 1)]
                    ti += 2
                    for _ in range(8):
                        next(gens[0])
                    while gens:
                        for g_ in list(gens):
                            try:
                                next(g_)
                            except StopIteration:
                                gens.remove(g_)
                    if p0g is not None:
                        for _ in range(3):
                            try:
                                next(p0g)
                            except StopIteration:
                                p0g = None
                                break
                if p0g is not None:
                    for _ in p0g:
                        pass
                C.barrier()

            with ExitStack() as ph:
                psum, PSB, psum_b, PSBB = mkps(ph, 6, 2)
                xg = [C.sb([128, D], BF16, ph) for _ in range(NST)]
                XG = bufs(NST)
                xgT2 = [[C.sb([128, KC, 128], BF16, ph) for _ in range(NST)] for _ in range(2)]
                XGT2 = [bufs(NST) for _ in range(2)]
                wgb = [C.sb([128, KC, 512], BF16, ph) for _ in range(4)]
                WGB = bufs(4)
                wdb = [C.sb([128, 8, 512], BF16, ph) for _ in range(4)]
                WDB = bufs(4)
                sgs = [C.sb([128, 512], F32, ph) for _ in range(2)]
                SGS = bufs(2)
                atok = [C.sb([128, EH], BF16, ph) for _ in range(NST)]
                ATOK = bufs(NST)
                aT = [C.sb([128, 8, 128], BF16, ph) for _ in range(NST)]
                AT = bufs(NST)
                ys = [C.sb([128, D], F32, ph) for _ in range(2)]
                YS = bufs(2)

                def load_x(ex):
                    for st in range(NST):
                        r0 = ex * CAP + st * 128
                        spq.dma(xg[st][:], slotbuf[r0:r0 + 128, :], reads=[SLOTB], writes=[XG[st]])

                def load_gu(ex, i2):
                    load_w(wgb[2 * i2][:], WGB[2 * i2], w_gu[l, ex, i2])
                    load_w(wgb[2 * i2 + 1][:], WGB[2 * i2 + 1], w_gu[l, ex, 2 + i2])

                def load_d(ex, n4):
                    load_w(wdb[n4][:], WDB[n4], w_dn[l, ex, n4])

                def prep(ex):
                    xgT, XGT = xgT2[ex % 2], XGT2[ex % 2]
                    for st in range(NST):
                        for g in range(2):
                            for kk in range(8):
                                k = g * 8 + kk
                                tr(psum_b[g][:, kk * 128:(kk + 1) * 128], PSBB[g], xg[st][:, k * 128:(k + 1) * 128], [XG[st]], ident_b, kk == 0)
                            src = psum_b[g][:, :].rearrange("p (a b) -> p a b", a=8)
                            if g == 0:
                                ACT.op(lambda e: e.copy(xgT[st][:, 0:8, :], src), [PSBB[g]], [XGT[st]])
                            else:
                                DVE.op(lambda e: e.tensor_copy(xgT[st][:, 8:16, :], src), [PSBB[g]], [XGT[st]])

                load_x(0)
                load_gu(0, 0)
                load_gu(0, 1)
                for n4 in range(4):
                    load_d(0, n4)
                for ex in range(NE):
                    nxt = ex + 1 < NE
                    xgT, XGT = xgT2[ex % 2], XGT2[ex % 2]
                    prep(ex)
                    if nxt:
                        load_x(ex + 1)
                    for i2 in range(2):
                        wg_, WG_ = wgb[2 * i2], WGB[2 * i2]
                        wu_, WU_ = wgb[2 * i2 + 1], WGB[2 * i2 + 1]
                        for st in range(NST):
                            sp_ = st % 2
                            pg_, PG_ = psum[2 * sp_], PSB[2 * sp_]
                            pu_, PU_ = psum[2 * sp_ + 1], PSB[2 * sp_ + 1]
                            for k in range(KC):
                                mm(pg_[:, :], PG_, xgT[st][:, k, :], [XGT[st]], wg_[:, k, :], [WG_], k == 0, k == KC - 1)
                            for k in range(KC):
                                mm(pu_[:, :], PU_, xgT[st][:, k, :], [XGT[st]], wu_[:, k, :], [WU_], k == 0, k == KC - 1)
                            ACT.op(lambda e: e.activation(out=sgs[sp_][:], in_=pg_[:, :], func=AF.Silu), [PG_], [SGS[sp_]])
                            DVE.op(lambda e: e.tensor_tensor(out=atok[st][:, i2 * 512:(i2 + 1) * 512], in0=pu_[:, :], in1=sgs[sp_][:], op=ALU.mult), [PU_, SGS[sp_]], [ATOK[st]])
                        if nxt:
                            load_gu(ex + 1, i2)
                    for st in range(NST):
                        sp_ = st % 2
                        for kk in range(8):
                            tr(psum_b[sp_][:, kk * 128:(kk + 1) * 128], PSBB[sp_], atok[st][:, kk * 128:(kk + 1) * 128], [ATOK[st]], ident_b, kk == 0)
                        src = psum_b[sp_][:, :].rearrange("p (a b) -> p a b", a=8)
                        if sp_ == 0:
                            ACT.op(lambda e: e.copy(aT[st][:], src), [PSBB[sp_]], [AT[st]])
                        else:
                            DVE.op(lambda e: e.tensor_copy(aT[st][:], src), [PSBB[sp_]], [AT[st]])
                    for st in range(NST):
                        sp_ = st % 2
                        for n4 in range(4):
                            wd_, WD_ = wdb[n4], WDB[n4]
                            pd_, PD_ = psum[4 + (n4 % 2)], PSB[4 + (n4 % 2)]
                            for k in range(8):
                                mm(pd_[:, :], PD_, aT[st][:, k, :], [AT[st]], wd_[:, k, :], [WD_], k == 0, k == 7)
                            if n4 % 2 == 0:
                                ACT.op(lambda e: e.copy(ys[sp_][:, n4 * 512:(n4 + 1) * 512], pd_[:, :]), [PD_], [YS[sp_]])
                            else:
                                DVE.op(lambda e: e.tensor_copy(ys[sp_][:, n4 * 512:(n4 + 1) * 512], pd_[:, :]), [PD_], [YS[sp_]])
                        r0 = ex * CAP + st * 128
                        spq.dma(yslot[r0:r0 + 128, :], ys[sp_][:], reads=[YS[sp_]], writes=[YSLOTB])
                    if nxt:
                        for n4 in range(4):
                            load_d(ex + 1, n4)
                C.barrier()

            with ExitStack() as ph:
                psum, PSB, psum_b, PSBB = mkps(ph, 6, 2)
                r0b = [C.sb([128, D], F32, ph) for _ in range(2)]
                r1b = [C.sb([128, D], F32, ph) for _ in range(2)]
                R0, R1 = bufs(2), bufs(2)
                comb = [C.sb([128, D], F32, ph) for _ in range(2)]
                COMB = bufs(2)
                hold = [C.sb([128, KC, 128], F32, ph) for _ in range(2)]
                HOLD = bufs(2)
                hn = [C.sb([128, KC, 128], F32, ph) for _ in range(2)]
                HN = bufs(2)
                sqf = C.sb([128, KC, 128], F32, ph)
                SQF = Buf()
                rsf = C.sb([128, 128], F32, ph)
                RSF = Buf()
                fin = C.sb([128, KC, 128], F32, ph)
                FIN = Buf()
                otok = [C.sb([128, D], F32, ph) for _ in range(2)]
                OTOK = bufs(2)
                g2row = [C.sb([128, D], F32, ph) for _ in range(2 if ctx_full else 1)]
                G2R = bufs(2)
                zt_ = C.sb([128, 128], F32, ph)
                ZT_ = Buf()
                xbc = [C.sb([128, 128], F32, ph) for _ in range(2)]
                XBC = bufs(2)
                POOL.op(lambda e: e.memset(zt_[:], 0.0), [], [ZT_])
                for r in range(2 if ctx_full else 1):
                    for g in range(4):
                        for kk in range(4):
                            k = g * 4 + kk
                            ACT.op(lambda e: e.activation(out=xbc[k % 2][:], in_=zt_[:], func=AF.Identity, bias=modT[:, l, 80 + k, r:r + 1], scale=1.0), [ZT_, MOD[l]], [XBC[k % 2]])
                            tr(psum[g][:, kk * 128:(kk + 1) * 128], PSB[g], xbc[k % 2][:], [XBC[k % 2]], ident_f, kk == 0)
                        DVE.op(lambda e: e.tensor_copy(g2row[r][:, g * 512:(g + 1) * 512], psum[g][:, :]), [PSB[g]], [G2R[r]])
                for ii, i in enumerate(tiles4):
                    r = 1 if i < 2 else 0
                    p = ii % 2
                    plq._issue(lambda e: e.indirect_dma_start(out=r0b[p][:, :], out_offset=None, in_=yslot[:, :], in_offset=bass.IndirectOffsetOnAxis(ap=sid[:, i, 0:1], axis=0)), [YSLOTB, SIDB[i]], [R0[p]])
                    plq._issue(lambda e: e.indirect_dma_start(out=r1b[p][:, :], out_offset=None, in_=yslot[:, :], in_offset=bass.IndirectOffsetOnAxis(ap=sid[:, i, 1:2], axis=0)), [YSLOTB, SIDB[i]], [R1[p]])
                    spq.dma(hold[p][:], hT3[:, :, i * 128:(i + 1) * 128], reads=htb([i]), writes=[HOLD[p]])
                    DVE.op(lambda e: e.tensor_scalar(out=comb[p][:], in0=r0b[p][:], scalar1=wts[:, i, 0:1], scalar2=None, op0=ALU.mult), [R0[p], SIDB[i]], [COMB[p]])
                    DVE.op(lambda e: e.scalar_tensor_tensor(out=comb[p][:], in0=r1b[p][:], scalar=wts[:, i, 1:2], in1=comb[p][:], op0=ALU.mult, op1=ALU.add), [R1[p], SIDB[i], COMB[p]], [COMB[p]])
                    DVE.op(lambda e: e.tensor_tensor(out=comb[p][:], in0=comb[p][:], in1=g2row[r][:], op=ALU.mult), [COMB[p], G2R[r]], [COMB[p]])
                    for g in range(4):
                        for kk in range(4):
                            k = g * 4 + kk
                            tr(psum[g][:, kk * 128:(kk + 1) * 128], PSB[g], comb[p][:, k * 128:(k + 1) * 128], [COMB[p]], ident_f, kk == 0)
                        src = psum[g][:, :].rearrange("p (a b) -> p a b", a=4)
                        DVE.op(lambda e: e.tensor_tensor(out=hn[p][:, g * 4:(g + 1) * 4, :], in0=src, in1=hold[p][:, g * 4:(g + 1) * 4, :], op=ALU.add), [PSB[g], HOLD[p]], [HN[p]])
                    if not last:
                        spq.dma(hT3[:, :, i * 128:(i + 1) * 128], hn[p][:], reads=[HN[p]], writes=htb([i]))
                    else:
                        ACT.op(lambda e: e.activation(out=sqf[:], in_=hn[p][:], func=AF.Square), [HN[p]], [SQF])
                        for k in range(KC):
                            mm(psum[4][:, 0:128], PSB[4], ones_f, [CST], sqf[:, k, :], [SQF], k == 0, k == KC - 1)
                        ACT.op(lambda e: e.activation(out=rsf[:], in_=psum[4][:, 0:128], func=AF.Sqrt, bias=eps6, scale=1.0 / D), [PSB[4]], [RSF])
                        DVE.op(lambda e: e.reciprocal(rsf[:], rsf[:]), [RSF], [RSF])
                        for k in range(KC):
                            DVE.op(lambda e: e.scalar_tensor_tensor(out=fin[:, k, :], in0=hn[p][:, k, :], scalar=fg[:, k:k + 1], in1=rsf[:], op0=ALU.mult, op1=ALU.mult), [HN[p], RSF, SMALL], [FIN])
                        for g in range(4):
                            for kk in range(4):
                                k = g * 4 + kk
                                tr(psum[g][:, kk * 128:(kk + 1) * 128], PSB[g], fin[:, k, :], [FIN], ident_f, kk == 0)
                            if g % 2:
                                ACT.op(lambda e: e.copy(otok[p][:, g * 512:(g + 1) * 512], psum[g][:, :]), [PSB[g]], [OTOK[p]])
                            else:
                                DVE.op(lambda e: e.tensor_copy(otok[p][:, g * 512:(g + 1) * 512], psum[g][:, :]), [PSB[g]], [OTOK[p]])
                        spq.dma(out[(i - 2) * 128:(i - 1) * 128, :], otok[p][:], reads=[OTOK[p]])
                C.barrier()
        if dbg and l == 0:
            spq.dma(dbg_h, hT, reads=htb(range(NT)))

    C.barrier()
    for t in spq.outstanding() + plq.outstanding():
        SP.wait(t)
    C.es.close()
    return nc


def _consts():
    c = np.zeros((128, 640 + 2 * NLAT), np.float32)
    c[:, 0:128] = np.eye(128, dtype=np.float32)
    c[:, 128:256] = 1.0
    p = np.arange(128)
    c[p ^ 16, 256 + p] = 1.0
    c[:, 384:512] = (p[:, None] < p[None, :]).astype(np.float32)
    c[:, 512:544] = (np.arange(32) * CAP).astype(np.float32)[None, :]
    c[:, 544] = 1e-6
    c[:, 545] = 1e-5
    tok = np.arange(NLAT)
    pos = np.stack([tok // 64, tok % 64], -1).astype(np.float32)
    inv = (10000.0 ** (-np.arange(16, dtype=np.float32) / 16)).astype(np.float32)
    dd = p % 64
    axis = dd // 32
    half = (dd % 32) // 16
    f = dd % 16
    ang = pos[:, axis].T.astype(np.float32) * inv[f][:, None]
    c[:, 640:640 + NLAT] = np.cos(ang)
    sgn = np.where(half == 0, -1.0, 1.0).astype(np.float32)[:, None]
    c[:, 640 + NLAT:] = np.sin(ang) * sgn
    return c


def _colT(v, k):
    return np.ascontiguousarray(np.asarray(v, np.float32).reshape(k, 128).T)


def _tile(w, cw):
    w = np.asarray(w, np.float32)
    lead = w.shape[:-2]
    K_, N_ = w.shape[-2:]
    nl = len(lead)
    w = w.reshape(*lead, K_ // 128, 128, N_ // cw, cw)
    perm = tuple(range(nl)) + (nl + 2, nl + 1, nl + 0, nl + 3)
    return np.ascontiguousarray(w.transpose(perm))


def make_shared(inp, n_layers=DEPTH, with_moe=True):
    L = n_layers
    f = lambda a: np.ascontiguousarray(np.asarray(a, dtype=np.float32))
    m = {}
    m["ada_w"] = _tile(inp["ada_w"][:L], 512)
    m["ada_bT"] = f(np.stack([_colT(inp["ada_b"][l], 96) for l in range(L)]))
    m["n1gT"] = f(np.stack([_colT(inp["norm1_g"][l], KC) for l in range(L)]))
    m["n2gT"] = f(np.stack([_colT(inp["norm2_g"][l], KC) for l in range(L)]))
    m["fgT"] = _colT(inp["final_g"], KC)
    m["w_in"] = _tile(inp["w_in"][:L], 128)
    m["bgT"] = f(np.stack([_colT(inp["b_gate"][l], 48) for l in range(L)]))
    m["dlam"] = f(np.asarray(inp["diff_lambda"][:L]).reshape(L, 256))
    m["sgT"] = f(np.asarray(inp["subln_g"][:L]).reshape(L, 128, 1))
    m["w_o"] = _tile(np.concatenate([inp["w_attn_out"][:L], inp["w_sc_out"][:L], inp["w_cf_out"][:L]], 1), 128)
    m["w_mix"] = _tile(inp["w_mix"][:L], 128)
    m["scwT"] = f(np.stack([np.asarray(inp["sc_conv_w"][l]).reshape(3, 4, 128).transpose(2, 1, 0) for l in range(L)]))
    m["cfwT"] = f(np.stack([np.asarray(inp["cf_dw_w"][l]).reshape(31, 4, 128).transpose(2, 1, 0) for l in range(L)]))
    m["cfbT"] = f(np.stack([_colT(inp["cf_dw_b"][l], 4) for l in range(L)]))
    m["lngT"] = f(np.stack([_colT(inp["cf_ln_g"][l], 4) for l in range(L)]))
    m["lnbT"] = f(np.stack([_colT(inp["cf_ln_b"][l], 4) for l in range(L)]))
    m["consts"] = _consts()
    if with_moe:
        m["wr"] = f(np.concatenate([inp["router_g_w"][:L], inp["router_e_w"][:L]], -1))
        m["rb"] = f(np.concatenate([inp["router_g_b"][:L], inp["router_e_b"][:L]], -1).reshape(L, 1, 36))
        m["w_gu"] = _tile(inp["exp_w_gu"][:L], 512)
        m["w_dn"] = _tile(inp["exp_w_down"][:L], 512)
    return m


def make_in_map(inp, b, n_layers=DEPTH, with_moe=True, shared=None):
    m = dict(shared if shared is not None else make_shared(inp, n_layers, with_moe))
    f = lambda a: np.ascontiguousarray(np.asarray(a, dtype=np.float32))
    m["xin"] = f(np.concatenate([inp["ctx"][b], inp["x"][b]], 0))
    m["cvec"] = f(np.stack([_colT(inp["c"][b], KC), _colT(inp["c_ctx"], KC)], -1))
    return m


def kernel(**inputs):
    inp = {k: np.asarray(v) for k, v in inputs.items()}
    nc = build()
    shared = make_shared(inp)
    in_maps = [make_in_map(inp, b, shared=shared) for b in range(NBATCH)]
    res = run_bass_kernel_spmd(nc, in_maps, core_ids=list(range(NBATCH)))
    return np.stack([np.asarray(r["out"], dtype=np.float32) for r in res.results], 0)
```

```python
import math
from contextlib import ExitStack
import numpy as np
import concourse.bass as bass
import concourse.mybir as mybir
from concourse.bass_utils import run_bass_kernel_spmd

F32 = mybir.dt.float32
BF16 = mybir.dt.bfloat16
I32 = mybir.dt.int32
AF = mybir.ActivationFunctionType
ALU = mybir.AluOpType
AX = mybir.AxisListType

D = 2048
KC = 16
NCTX = 256
NLAT = 2048
T = NCTX + NLAT
NT = T // 128
DEPTH = 2
QK_W = 1024
OFF_Q, OFF_K, OFF_V, OFF_SC, OFF_CF, OFF_GATE = 0, 1024, 2048, 3072, 4608, 5632
C_TOT = 11776
NE = 32
CAP = 512
NST = CAP // 128
EH = 1024
NBATCH = 4
BIG = 1.0e30


class Buf:
    __slots__ = ("w", "r")

    def __init__(self):
        self.w = None
        self.r = {}


def bufs(n):
    return [Buf() for _ in range(n)]


class Eng:
    LIMIT = 30000

    def __init__(self, ctx, name, e):
        self.ctx, self.name, self.e = ctx, name, e
        self.nsem = 0
        self.waited = {}
        self.sem = None
        self.cnt = 0
        self._newsem()

    def _newsem(self):
        self.sem = self.ctx.sem(f"{self.name}_s{self.nsem}")
        self.nsem += 1
        self.cnt = 0

    def wait(self, tok):
        if tok is None:
            return
        s, v = tok
        if self.waited.get(id(s), 0) >= v:
            return
        self.e.wait_ge(s, v)
        self.waited[id(s)] = v

    def deps(self, reads, writes):
        for b in reads:
            self.wait(b.w)
        for b in writes:
            self.wait(b.w)
            for t in list(b.r.values()):
                self.wait(t)

    @staticmethod
    def done(tok, reads, writes):
        for b in reads:
            b.r[id(tok[0])] = tok
        for b in writes:
            b.w = tok
            b.r = {}

    def op(self, fn, reads=(), writes=(), skip_out_deps=False):
        self.deps(reads, () if skip_out_deps else writes)
        if self.cnt >= self.LIMIT:
            self._newsem()
        ins = fn(self.e)
        self.cnt += 1
        ins.then_inc(self.sem, 1)
        tok = (self.sem, self.cnt)
        self.done(tok, reads, writes)
        self.ctx.last[self.name] = tok
        return tok


class DmaQ:
    def __init__(self, ctx, name, eng, npool=8):
        self.ctx, self.eng = ctx, eng
        self.pool = [ctx.sem(f"{name}_d{i}") for i in range(npool)]
        self.vals = [0] * npool
        self.i = 0

    def _issue(self, fn, reads, writes):
        E = self.eng
        E.deps(reads, writes)
        i = self.i
        s = self.pool[i]
        if self.vals[i] > 0:
            E.wait((s, self.vals[i]))
        ins = fn(E.e)
        self.vals[i] += 16
        ins.then_inc(s, 16)
        tok = (s, self.vals[i])
        self.i = (i + 1) % len(self.pool)
        Eng.done(tok, reads, writes)
        return tok

    def dma(self, out, in_, reads=(), writes=(), **kw):
        return self._issue(lambda e: e.dma_start(out=out, in_=in_, **kw), reads, writes)

    def outstanding(self):
        return [(s, v) for s, v in zip(self.pool, self.vals) if v > 0]


class Ctx:
    def __init__(self, nc):
        self.nc = nc
        self.es = ExitStack()
        self.last = {}
        self.nsb = 0
        self.PE = Eng(self, "pe", nc.tensor)
        self.ACT = Eng(self, "act", nc.scalar)
        self.DVE = Eng(self, "dve", nc.vector)
        self.POOL = Eng(self, "pool", nc.gpsimd)
        self.SP = Eng(self, "sp", nc.sync)
        self.spq = DmaQ(self, "spq", self.SP, 12)
        self.plq = DmaQ(self, "plq", self.POOL, 8)
        self.engs = [self.PE, self.ACT, self.DVE, self.POOL, self.SP]

    def sem(self, name):
        return self.es.enter_context(self.nc.semaphore(name))

    def sb(self, shape, dt, stack=None, name=None):
        self.nsb += 1
        st = stack if stack is not None else self.es
        return st.enter_context(self.nc.sbuf_tensor(name or f"sb{self.nsb}", list(shape), dt))

    def barrier(self):
        toks = list(self.last.values()) + self.spq.outstanding() + self.plq.outstanding()
        for E in self.engs:
            for t in toks:
                E.wait(t)


def build(n_layers=DEPTH, with_moe=True, dbg=False):
    nc = bass.Bass("TRN2", target_bir_lowering=False)
    C = Ctx(nc)
    PE, ACT, DVE, POOL, SP = C.PE, C.ACT, C.DVE, C.POOL, C.SP
    spq, plq = C.spq, C.plq

    def din(name, shape, dt=F32):
        return nc.dram_tensor(name, list(shape), dt, kind="ExternalInput").ap()

    xin = din("xin", [T, D])
    cvec = din("cvec", [128, KC, 2])
    ada_w = din("ada_w", [n_layers, 24, 128, KC, 512])
    ada_bT = din("ada_bT", [n_layers, 128, 96])
    n1gT = din("n1gT", [n_layers, 128, KC])
    n2gT = din("n2gT", [n_layers, 128, KC])
    fgT = din("fgT", [128, KC])
    w_in = din("w_in", [n_layers, C_TOT // 128, 128, KC, 128])
    bgT = din("bgT", [n_layers, 128, 48])
    dlam = din("dlam", [n_layers, 256])
    sgT = din("sgT", [n_layers, 128, 1])
    w_o_in = din("w_o", [n_layers, KC, 128, KC, 128])
    w_mix = din("w_mix", [n_layers, KC, 128, KC, 128])
    scwT = din("scwT", [n_layers, 128, 4, 3])
    cfwT = din("cfwT", [n_layers, 128, 4, 31])
    cfbT = din("cfbT", [n_layers, 128, 4])
    lngT = din("lngT", [n_layers, 128, 4])
    lnbT = din("lnbT", [n_layers, 128, 4])
    consts = din("consts", [128, 640 + 2 * NLAT])
    if with_moe:
        wr = din("wr", [n_layers, D, 36])
        rb = din("rb", [n_layers, 1, 36])
        w_gu = din("w_gu", [n_layers, NE, 4, 128, KC, 512])
        w_dn = din("w_dn", [n_layers, NE, 4, 128, 8, 512])
    out = nc.dram_tensor("out", [NLAT, D], F32, kind="ExternalOutput").ap()
    dbg_h = nc.dram_tensor("dbg_h", [D, T], F32, kind="ExternalOutput").ap() if dbg else None
    dbg_br = nc.dram_tensor("dbg_br", [D, T], BF16, kind="ExternalOutput").ap() if dbg else None

    hT = nc.dram_tensor("hT", [D, T], F32, kind="Internal").ap()
    brT = nc.dram_tensor("brT", [D, T], BF16, kind="Internal").ap()
    if with_moe:
        slotbuf = nc.dram_tensor("slotbuf", [NE * CAP, D], BF16, kind="Internal").ap()
        yslot = nc.dram_tensor("yslot", [NE * CAP, D], F32, kind="Internal").ap()
    hT3 = hT.rearrange("(k p) t -> p k t", p=128)
    brT3 = brT.rearrange("(k p) t -> p k t", p=128)
    HTBK = [bufs(NT) for _ in range(KC)]

    def htb(tiles, k=None):
        ks = range(KC) if k is None else [k]
        return [HTBK[k_][i_] for k_ in ks for i_ in tiles]
    BRB = [bufs(NT) for _ in range(KC)]
    SLOTB, YSLOTB = Buf(), Buf()

    cst = C.sb([128, 640], F32, name="cst")
    CST = Buf()
    spq.dma(cst[:], consts[:, 0:640], writes=[CST])
    ident_f = cst[:, 0:128]
    ones_f = cst[:, 128:256]
    rperm_f = cst[:, 256:384]
    tri_f = cst[:, 384:512]
    ecap_f = cst[:, 512:544]
    eps6 = cst[:, 544:545]
    eps5 = cst[:, 545:546]
    cb = C.sb([128, 384], BF16, name="cb")
    CB = Buf()
    DVE.op(lambda e: e.tensor_copy(cb[:, 0:128], ident_f), [CST], [CB])
    DVE.op(lambda e: e.tensor_copy(cb[:, 128:256], ones_f), [CST], [CB])
    DVE.op(lambda e: e.tensor_copy(cb[:, 256:384], tri_f), [CST], [CB])
    ident_b, ones_b, tri_b = cb[:, 0:128], cb[:, 128:256], cb[:, 256:384]

    modT = C.sb([128, n_layers, 96, 2], F32, name="modT")
    A1 = C.sb([128, n_layers, KC, 2], F32, name="A1")
    A2 = C.sb([128, n_layers, KC, 2], F32, name="A2")
    MOD = bufs(n_layers)
    small = C.sb([128, n_layers, 160], F32, name="small")
    cfw = C.sb([128, n_layers, 4 * 31 + 12 + 12], F32, name="cfw")
    fg = C.sb([128, KC], F32, name="fg")
    lamt = C.sb([128, n_layers, 4], F32, name="lamt")
    SMALL = Buf()
    for l in range(n_layers):
        spq.dma(small[:, l, 0:16], n1gT[l], writes=[SMALL])
        spq.dma(small[:, l, 16:32], n2gT[l], writes=[SMALL])
        spq.dma(small[:, l, 32:80], bgT[l], writes=[SMALL])
        spq.dma(small[:, l, 80:81], sgT[l], writes=[SMALL])
        spq.dma(cfw[:, l, 0:124], cfwT[l].rearrange("p c k -> p (c k)"), writes=[SMALL])
        spq.dma(cfw[:, l, 124:128], cfbT[l], writes=[SMALL])
        spq.dma(cfw[:, l, 128:132], lngT[l], writes=[SMALL])
        spq.dma(cfw[:, l, 132:136], lnbT[l], writes=[SMALL])
        spq.dma(cfw[:, l, 136:148], scwT[l].rearrange("p c k -> p (c k)"), writes=[SMALL])
    spq.dma(fg[:], fgT, writes=[SMALL])

    pscnt = [0]

    def mkps(stack, nf=6, nb=2):
        pscnt[0] += 1
        t = pscnt[0]
        ps_f = [stack.enter_context(nc.psum_tensor(f"ps{t}_{i}", [128, 512], F32)) for i in range(nf)]
        ps_b = [stack.enter_context(nc.psum_tensor(f"psb{t}_{i}", [128, 1024], BF16)) for i in range(nb)]
        return ps_f, bufs(nf), ps_b, bufs(nb)

    def mm(out_ap, ob, lhsT, lb, rhs, rb_, start, stop):
        PE.op(lambda e: e.matmul(out_ap, lhsT, rhs, start=start, stop=stop),
              list(lb) + list(rb_), [ob], skip_out_deps=not start)

    def tr(out_ap, ob, in_ap, ib, idn, first):
        PE.op(lambda e: e.transpose(out_ap, in_ap, idn), list(ib) + [CST, CB], [ob], skip_out_deps=not first)

    def load_w(dst3, WB, src3):
        return plq.dma(dst3, src3, writes=[WB])

    def p0_gen(l, ph, pbanks, cw=512):
        ncc = cw // 128
        nit = (6 * D) // cw
        cv = C.sb([128, KC, 2], F32, ph)
        sT = C.sb([128, KC, 2], BF16, ph)
        CV, ST = Buf(), Buf()
        adab = C.sb([128, 96], F32, ph)
        ADAB = Buf()
        wt = [C.sb([128, KC, cw], BF16, ph) for _ in range(2)]
        WT = bufs(2)
        lv = C.sb([128, 256], F32, ph)
        LV = Buf()
        pr = C.sb([128, 2, 64], F32, ph)
        PR = Buf()
        sm = C.sb([128, 4], F32, ph)
        SM = Buf()
        spq.dma(cv[:], cvec, writes=[CV])
        spq.dma(adab[:], ada_bT[l], writes=[ADAB])
        spq.dma(lv[:], dlam[l].partition_broadcast(128), writes=[LV])
        ACT.op(lambda e: e.activation(out=sT[:], in_=cv[:], func=AF.Silu), [CV], [ST])
        pend = None

        def evac(it_, pb, PB_):
            src = pb[:, 0:2 * ncc].rearrange("p (a b) -> p a b", a=ncc)
            ACT.op(lambda e: e.copy(modT[:, l, it_ * ncc:(it_ + 1) * ncc, :], src), [PB_], [MOD[l]])

        for it_ in range(nit):
            w_, W_ = wt[it_ % 2], WT[it_ % 2]
            pb, PB_ = pbanks[it_ % 2]
            c0 = it_ * cw
            cg, co = c0 // 512, c0 % 512
            load_w(w_[:], W_, ada_w[l, cg][:, :, co:co + cw])
            if pend is not None:
                evac(*pend)
            for cc in range(ncc):
                for k in range(KC):
                    mm(pb[:, cc * 2:cc * 2 + 2], PB_, w_[:, k, cc * 128:(cc + 1) * 128], [W_], sT[:, k, :], [ST], k == 0, k == KC - 1)
            pend = (it_, pb, PB_)
            yield
        evac(*pend)
        for r in range(2):
            DVE.op(lambda e: e.tensor_tensor(out=modT[:, l, :, r], in0=modT[:, l, :, r], in1=adab[:], op=ALU.add), [MOD[l], ADAB], [MOD[l]])
            DVE.op(lambda e: e.scalar_tensor_tensor(out=A1[:, l, :, r], in0=modT[:, l, 16:32, r], scalar=1.0, in1=small[:, l, 0:16], op0=ALU.add, op1=ALU.mult), [MOD[l], SMALL], [MOD[l]])
            DVE.op(lambda e: e.scalar_tensor_tensor(out=A2[:, l, :, r], in0=modT[:, l, 64:80, r], scalar=1.0, in1=small[:, l, 16:32], op0=ALU.add, op1=ALU.mult), [MOD[l], SMALL], [MOD[l]])
        yield
        lam_init = 0.8 - 0.6 * math.exp(-0.3 * l)
        DVE.op(lambda e: e.tensor_tensor(out=pr[:, 0, :], in0=lv[:, 0:64], in1=lv[:, 64:128], op=ALU.mult), [LV], [PR])
        DVE.op(lambda e: e.tensor_tensor(out=pr[:, 1, :], in0=lv[:, 128:192], in1=lv[:, 192:256], op=ALU.mult), [LV], [PR])
        DVE.op(lambda e: e.tensor_reduce(out=sm[:, 0:2], in_=pr[:], axis=AX.X, op=ALU.add), [PR], [SM])
        ACT.op(lambda e: e.activation(out=sm[:, 2:4], in_=sm[:, 0:2], func=AF.Exp), [SM], [SM])
        DVE.op(lambda e: e.scalar_tensor_tensor(out=lamt[:, l, 0:1], in0=sm[:, 3:4], scalar=-lam_init, in1=sm[:, 2:3], op0=ALU.add, op1=ALU.subtract), [SM], [MOD[l]])
        DVE.op(lambda e: e.tensor_scalar(out=lamt[:, l, 1:2], in0=small[:, l, 80:81], scalar1=(1.0 - lam_init), scalar2=None, op0=ALU.mult), [SMALL], [MOD[l]])
        yield

    with ExitStack() as ph:
        psum, PSB, psum_b, PSBB = mkps(ph, 6, 2)
        xb = [C.sb([128, D], F32, ph) for _ in range(2)]
        XB = bufs(2)
        stg = [C.sb([128, KC, 128], F32, ph) for _ in range(2)]
        STG = bufs(2)
        p0gs = [p0_gen(0, ph, [(psum[4], PSB[4]), (psum[5], PSB[5])])]

        def p0_step(n_):
            for _ in range(n_):
                while p0gs:
                    try:
                        next(p0gs[0])
                        break
                    except StopIteration:
                        p0gs.pop(0)
        for i in range(NT):
            x_, X_ = xb[i % 2], XB[i % 2]
            s_, S_ = stg[i % 2], STG[i % 2]
            spq.dma(x_[:], xin[i * 128:(i + 1) * 128, :], writes=[X_])
            for g in range(4):
                for kk in range(4):
                    k = g * 4 + kk
                    tr(psum[g][:, kk * 128:(kk + 1) * 128], PSB[g], x_[:, k * 128:(k + 1) * 128], [X_], ident_f, kk == 0)
                E = ACT if g % 2 else DVE
                src = psum[g][:, :].rearrange("p (a b) -> p a b", a=4)
                if E is ACT:
                    E.op(lambda e: e.copy(s_[:, g * 4:(g + 1) * 4, :], src), [PSB[g]], [S_])
                else:
                    E.op(lambda e: e.tensor_copy(s_[:, g * 4:(g + 1) * 4, :], src), [PSB[g]], [S_])
            spq.dma(hT3[:, :, i * 128:(i + 1) * 128], s_[:], reads=[S_], writes=htb([i]))
            p0_step(3)
        p0_step(100)
        C.barrier()


    def col(ap4, l, j, r):
        return ap4[:, l, j, r:r + 1]

    UTB = [bufs(KC) for _ in range(9)]

    def ut_bufs(t0, n):
        cs = range(t0 // 256, (t0 + n + 255) // 256)
        return [UTB[c][k] for c in cs for k in range(KC)]

    def ut_bufs_k(t0, n, k):
        return [UTB[c][k] for c in range(t0 // 256, (t0 + n + 255) // 256)]

    for l in range(n_layers):
        last = l == n_layers - 1 and n_layers == DEPTH
        lastl = l == n_layers - 1
        ctx_full = not (l == DEPTH - 1)
        w_in_l = w_in[l]
        phU = ExitStack()
        uT = C.sb([128, KC, T], BF16, phU, name=f"uT{l}")
        with ExitStack() as ph:
            psum, PSB, psum_b, PSBB = mkps(ph, 6, 2)
            hcb = [C.sb([128, KC, 256], F32, ph) for _ in range(2)]
            HCB = bufs(2)
            sq = C.sb([128, KC, 256], F32, ph)
            SQ = Buf()
            rstd = [C.sb([128, 256], F32, ph) for _ in range(2)]
            RS = bufs(2)
            tmp = [C.sb([128, 256], F32, ph) for _ in range(2)]
            TMP = bufs(2)
            for c in range(9):
                r = 1 if c == 0 else 0
                hc, HC = hcb[c % 2], HCB[c % 2]
                rs_, RS_ = rstd[c % 2], RS[c % 2]
                spq.dma(hc[:], hT3[:, :, c * 256:(c + 1) * 256], reads=htb([2 * c, 2 * c + 1]), writes=[HC])
                ACT.op(lambda e: e.activation(out=sq[:], in_=hc[:], func=AF.Square), [HC], [SQ])
                pb, PB_ = psum[c % 2], PSB[c % 2]
                for k in range(KC):
                    mm(pb[:, 0:256], PB_, ones_f, [CST], sq[:, k, :], [SQ], k == 0, k == KC - 1)
                ACT.op(lambda e: e.activation(out=rs_[:], in_=pb[:, 0:256], func=AF.Sqrt, bias=eps6, scale=1.0 / D), [PB_], [RS_])
                DVE.op(lambda e: e.reciprocal(rs_[:], rs_[:]), [RS_], [RS_])
                for k in range(KC):
                    t_, T_ = tmp[k % 2], TMP[k % 2]
                    DVE.op(lambda e: e.scalar_tensor_tensor(out=t_[:], in0=hc[:, k, :], scalar=col(A1, l, k, r), in1=rs_[:], op0=ALU.mult, op1=ALU.mult), [HC, RS_, MOD[l]], [T_])
                    ACT.op(lambda e: e.activation(out=uT[:, k, c * 256:(c + 1) * 256], in_=t_[:], func=AF.Identity, bias=modT[:, l, 0 + k, r:r + 1], scale=1.0), [T_, MOD[l]], [UTB[c][k]])
            C.barrier()

        with ExitStack() as ph:
            psum, PSB, psum_b, PSBB = mkps(ph, 8, 0)
            rope = C.sb([128, 2 * NLAT], F32, ph)
            ROPE = Buf()
            spq.dma(rope[:], consts[:, 640:640 + 2 * NLAT], writes=[ROPE])
            cosT = rope[:, 0:NLAT]
            sinT = rope[:, NLAT:2 * NLAT]
            wq = [C.sb([128, KC, 128], BF16, ph) for _ in range(2)]
            wk = [C.sb([128, KC, 128], BF16, ph) for _ in range(2)]
            wv = [C.sb([128, KC, 128], BF16, ph) for _ in range(2)]
            WQ, WK, WV = bufs(2), bufs(2), bufs(2)
            QT = [C.sb([128, T], BF16, ph) for _ in range(2)]
            KT = [C.sb([128, T], BF16, ph) for _ in range(2)]
            VH = [C.sb([128, NT, 128], BF16, ph) for _ in range(2)]
            QTB = [bufs(5) for _ in range(2)]
            KTB = [bufs(5) for _ in range(2)]
            VHB = [bufs(NT) for _ in range(2)]
            qf = [C.sb([128, 512], F32, ph) for _ in range(2)]
            QF = bufs(2)
            t1 = [C.sb([128, 512], F32, ph) for _ in range(2)]
            T1 = bufs(2)
            t2 = [C.sb([128, 512], F32, ph) for _ in range(2)]
            T2 = bufs(2)
            ET = [C.sb([128, 512], BF16, ph) for _ in range(6)]
            ETB = bufs(6)
            am = 0
            rd = C.sb([128, 512], F32, ph)
            RD = Buf()
            om = [C.sb([128, 512], F32, ph) for _ in range(2)]
            OM = bufs(2)
            oo = C.sb([128, 512], F32, ph)
            OO = Buf()
            sqo = C.sb([128, 512], F32, ph)
            SQO = Buf()
            rs2 = C.sb([128, 512], F32, ph)
            RS2 = Buf()
            aTh = [C.sb([128, T], BF16, ph) for _ in range(2)]
            ATH = [bufs(5) for _ in range(2)]
            chunks = [(0, 256, True)] + [(256 + 512 * i, 512, False) for i in range(4)]

            def chunk_idx(t0):
                return 0 if t0 == 0 else 1 + (t0 - 256) // 512

            for h in range(8):
                hp = h % 2
                load_w(wq[hp][:], WQ[hp], w_in_l[OFF_Q // 128 + h])
                load_w(wk[hp][:], WK[hp], w_in_l[OFF_K // 128 + h])
                load_w(wv[hp][:], WV[hp], w_in_l[OFF_V // 128 + h])
                pi = 0
                for (w_, W_, dst, DSTB, need_ctx) in ((wq[hp], WQ[hp], QT[hp], QTB[hp], ctx_full), (wk[hp], WK[hp], KT[hp], KTB[hp], True)):
                    for (t0, n, isctx) in chunks:
                        if isctx and not need_ctx:
                            continue
                        ci = chunk_idx(t0)
                        pb, PB_ = psum[3], PSB[3]
                        for k in range(KC):
                            mm(pb[:, 0:n], PB_, w_[:, k, :], [W_], uT[:, k, t0:t0 + n], ut_bufs_k(t0, n, k), k == 0, k == KC - 1)
                        if isctx:
                            ACT.op(lambda e: e.copy(dst[:, t0:t0 + n], pb[:, 0:n]), [PB_], [DSTB[ci]])
                        else:
                            q_, Q_ = qf[pi % 2], QF[pi % 2]
                            a_, A_ = t1[pi % 2], T1[pi % 2]
                            b_, B_ = t2[pi % 2], T2[pi % 2]
                            pi += 1
                            l0 = t0 - NCTX
                            ACT.op(lambda e: e.copy(q_[:], pb[:, 0:n]), [PB_], [Q_])
                            mm(psum[4][:, 0:n], PSB[4], rperm_f, [CST], q_[:], [Q_], True, True)
                            POOL.op(lambda e: e.tensor_tensor(out=a_[:], in0=q_[:], in1=cosT[:, l0:l0 + n], op=ALU.mult), [Q_, ROPE], [A_])
                            DVE.op(lambda e: e.tensor_tensor(out=b_[:], in0=psum[4][:, 0:n], in1=sinT[:, l0:l0 + n], op=ALU.mult), [PSB[4], ROPE], [B_])
                            DVE.op(lambda e: e.tensor_tensor(out=dst[:, t0:t0 + n], in0=a_[:], in1=b_[:], op=ALU.add), [A_, B_], [DSTB[ci]])
                for g in range((NT + 3) // 4):
                    tiles = list(range(g * 4, min(NT, g * 4 + 4)))
                    pb, PB_ = psum[3], PSB[3]
                    for ii, i in enumerate(tiles):
                        for k in range(KC):
                            mm(pb[:, ii * 128:(ii + 1) * 128], PB_, uT[:, k, i * 128:(i + 1) * 128], ut_bufs_k(i * 128, 128, k), wv[hp][:, k, :], [WV[hp]], k == 0, k == KC - 1)
                    nt_ = len(tiles)
                    src = pb[:, 0:nt_ * 128].rearrange("p (a b) -> p a b", a=nt_)
                    ACT.op(lambda e: e.copy(VH[hp][:, g * 4:g * 4 + nt_, :], src), [PB_], [VHB[hp][i] for i in tiles])
                for (q0, n, isctx) in chunks:
                    if isctx and not ctx_full:
                        continue
                    ci = chunk_idx(q0)
                    kts = [0, 1] if isctx else list(range(NT))
                    for m in range(2):
                        pr_ = slice(m * 64, (m + 1) * 64)
                        pO, PO_ = psum[4 + am % 2], PSB[4 + am % 2]
                        pD, PD_ = psum[6 + am % 2], PSB[6 + am % 2]
                        am += 1

                        def S(j):
                            kt = kts[j]
                            kci = chunk_idx(0 if kt < 2 else 256 + ((kt - 2) // 4) * 512)
                            mm(psum[j % 4][:, 0:n], PSB[j % 4], KT[hp][pr_, kt * 128:(kt + 1) * 128], [KTB[hp][kci]], QT[hp][pr_, q0:q0 + n], [QTB[hp][ci]], True, True)
                            ACT.op(lambda e: e.activation(out=ET[j % 6][:, 0:n], in_=psum[j % 4][:, 0:n], func=AF.Exp, scale=0.125), [PSB[j % 4]], [ETB[j % 6]])

                        def OD(j):
                            kt = kts[j]
                            mm(pO[:, 0:n], PO_, VH[hp][:, kt, :], [VHB[hp][kt]], ET[j % 6][:, 0:n], [ETB[j % 6]], j == 0, j == len(kts) - 1)
                            mm(pD[:, 0:n], PD_, ones_b, [CB], ET[j % 6][:, 0:n], [ETB[j % 6]], j == 0, j == len(kts) - 1)

                        for j in range(min(3, len(kts))):
                            S(j)
                        for j in range(len(kts)):
                            if j + 3 < len(kts):
                                S(j + 3)
                            OD(j)
                        DVE.op(lambda e: e.reciprocal(rd[:, 0:n], pD[:, 0:n]), [PD_], [RD])
                        DVE.op(lambda e: e.tensor_tensor(out=om[m][:, 0:n], in0=pO[:, 0:n], in1=rd[:, 0:n], op=ALU.mult), [PO_, RD], [OM[m]])
                    DVE.op(lambda e: e.scalar_tensor_tensor(out=oo[:, 0:n], in0=om[1][:, 0:n], scalar=lamt[:, l, 0:1], in1=om[0][:, 0:n], op0=ALU.mult, op1=ALU.add), [OM[0], OM[1], MOD[l]], [OO])
                    ACT.op(lambda e: e.activation(out=sqo[:, 0:n], in_=oo[:, 0:n], func=AF.Square), [OO], [SQO])
                    mm(psum[5][:, 0:n], PSB[5], ones_f, [CST], sqo[:, 0:n], [SQO], True, True)
                    ACT.op(lambda e: e.activation(out=rs2[:, 0:n], in_=psum[5][:, 0:n], func=AF.Sqrt, bias=eps5, scale=1.0 / 128), [PSB[5]], [RS2])
                    DVE.op(lambda e: e.reciprocal(rs2[:, 0:n], rs2[:, 0:n]), [RS2], [RS2])
                    DVE.op(lambda e: e.scalar_tensor_tensor(out=aTh[hp][:, q0:q0 + n], in0=oo[:, 0:n], scalar=lamt[:, l, 1:2], in1=rs2[:, 0:n], op0=ALU.mult, op1=ALU.mult), [OO, RS2, MOD[l]], [ATH[hp][ci]])
                    tl = list(range(q0 // 128, (q0 + n) // 128))
                    spq.dma(brT[h * 128:(h + 1) * 128, q0:q0 + n], aTh[hp][:, q0:q0 + n], reads=[ATH[hp][ci]], writes=[BRB[h][i] for i in tl])
            C.barrier()

        with ExitStack() as ph:
            psum, PSB, psum_b, PSBB = mkps(ph, 8, 0)
            p0n = [p0_gen(l + 1, ph, [(psum[6], PSB[6]), (psum[7], PSB[7])], cw=128)] if l + 1 < n_layers else []

            def p0s(n_):
                for _ in range(n_):
                    if not p0n:
                        return
                    try:
                        next(p0n[0])
                    except StopIteration:
                        p0n.pop()

            wa = [C.sb([128, KC, 128], BF16, ph) for _ in range(6)]
            WA = bufs(6)
            seqs = [(NCTX, NLAT)] + ([(0, NCTX)] if ctx_full else [])
            zb = C.sb([128, 4, NLAT], F32, ph)
            ZB = bufs(4)
            cfin = C.sb([128, NLAT + 30], F32, ph)
            CFIN = Buf()
            sg = [C.sb([128, 512], F32, ph) for _ in range(2)]
            SG = bufs(2)
            sqz = C.sb([128, 4, 512], F32, ph)
            SQZ = Buf()
            mean = C.sb([128, 512], F32, ph)
            MEAN = Buf()
            msq = C.sb([128, 512], F32, ph)
            MSQ = Buf()
            var = C.sb([128, 512], F32, ph)
            VAR = Buf()
            zt = [C.sb([128, 512], F32, ph) for _ in range(2)]
            ZT = bufs(2)
            cto = [C.sb([128, 512], BF16, ph) for _ in range(2)]
            CTO = bufs(2)
            bsave = zb[:, 0, :]
            BSAVE = ZB[0]
            ysc = zb[:, 1, :]
            YSC = ZB[1]
            bto = C.sb([128, NLAT], BF16, ph)
            BTO = Buf()
            wi = 0
            for (s0, L) in seqs:
                nch = [(o, min(512, L - o)) for o in range(0, L, 512)]
                for c in range(4):
                    wa_, WA_ = wa[wi % 6], WA[wi % 6]
                    wg_, WG_ = wa[(wi + 1) % 6], WA[(wi + 1) % 6]
                    wi += 2
                    load_w(wa_[:], WA_, w_in_l[OFF_CF // 128 + c])
                    load_w(wg_[:], WG_, w_in_l[OFF_CF // 128 + 4 + c])
                    POOL.op(lambda e: e.memset(cfin[:, 0:15], 0.0), [], [CFIN])
                    POOL.op(lambda e: e.memset(cfin[:, 15 + L:30 + L], 0.0), [], [CFIN])
                    for ii, (o, n) in enumerate(nch):
                        t0 = s0 + o
                        pa, PA_ = psum[0 + 2 * (ii % 2)], PSB[0 + 2 * (ii % 2)]
                        pg, PG_ = psum[1 + 2 * (ii % 2)], PSB[1 + 2 * (ii % 2)]
                        for k in range(KC):
                            mm(pa[:, 0:n], PA_, wa_[:, k, :], [WA_], uT[:, k, t0:t0 + n], ut_bufs_k(t0, n, k), k == 0, k == KC - 1)
                        for k in range(KC):
                            mm(pg[:, 0:n], PG_, wg_[:, k, :], [WG_], uT[:, k, t0:t0 + n], ut_bufs_k(t0, n, k), k == 0, k == KC - 1)
                        s_, S_ = sg[ii % 2], SG[ii % 2]
                        ACT.op(lambda e: e.activation(out=s_[:, 0:n], in_=pg[:, 0:n], func=AF.Sigmoid), [PG_], [S_])
                        DVE.op(lambda e: e.tensor_tensor(out=cfin[:, 15 + o:15 + o + n], in0=pa[:, 0:n], in1=s_[:, 0:n], op=ALU.mult), [PA_, S_], [CFIN])
                    p0s(7)
                    wb0 = c * 31
                    DVE.op(lambda e: e.tensor_scalar(out=zb[:, c, 0:L], in0=cfin[:, 0:L], scalar1=cfw[:, l, wb0:wb0 + 1], scalar2=cfw[:, l, 124 + c:125 + c], op0=ALU.mult, op1=ALU.add), [CFIN, SMALL], [ZB[c]])
                    for kk in range(1, 31):
                        E = DVE
                        E.op(lambda e: e.scalar_tensor_tensor(out=zb[:, c, 0:L], in0=cfin[:, kk:kk + L], scalar=cfw[:, l, wb0 + kk:wb0 + kk + 1], in1=zb[:, c, 0:L], op0=ALU.mult, op1=ALU.add), [CFIN, SMALL], [ZB[c]])
                for ii, (o, n) in enumerate(nch):
                    t0 = s0 + o
                    ACT.op(lambda e: e.activation(out=sqz[:, :, 0:n], in_=zb[:, :, o:o + n], func=AF.Square), ZB, [SQZ])
                    for c in range(4):
                        mm(psum[0][:, 0:n], PSB[0], ones_f, [CST], zb[:, c, o:o + n], [ZB[c]], c == 0, c == 3)
                    for c in range(4):
                        mm(psum[1][:, 0:n], PSB[1], ones_f, [CST], sqz[:, c, 0:n], [SQZ], c == 0, c == 3)
                    ACT.op(lambda e: e.activation(out=mean[:, 0:n], in_=psum[0][:, 0:n], func=AF.Identity, scale=1.0 / 512), [PSB[0]], [MEAN])
                    DVE.op(lambda e: e.tensor_tensor(out=msq[:, 0:n], in0=mean[:, 0:n], in1=mean[:, 0:n], op=ALU.mult), [MEAN], [MSQ])
                    DVE.op(lambda e: e.scalar_tensor_tensor(out=var[:, 0:n], in0=psum[1][:, 0:n], scalar=1.0 / 512, in1=msq[:, 0:n], op0=ALU.mult, op1=ALU.subtract), [PSB[1], MSQ], [VAR])
                    ACT.op(lambda e: e.activation(out=var[:, 0:n], in_=var[:, 0:n], func=AF.Sqrt, bias=eps5, scale=1.0), [VAR], [VAR])
                    DVE.op(lambda e: e.reciprocal(var[:, 0:n], var[:, 0:n]), [VAR], [VAR])
                    for c in range(4):
                        z_, Z_ = zt[c % 2], ZT[c % 2]
                        o_, O_ = cto[c % 2], CTO[c % 2]
                        DVE.op(lambda e: e.tensor_tensor(out=z_[:, 0:n], in0=zb[:, c, o:o + n], in1=mean[:, 0:n], op=ALU.subtract), [ZB[c], MEAN], [Z_])
                        DVE.op(lambda e: e.tensor_tensor(out=z_[:, 0:n], in0=z_[:, 0:n], in1=var[:, 0:n], op=ALU.mult), [Z_, VAR], [Z_])
                        ACT.op(lambda e: e.activation(out=o_[:, 0:n], in_=z_[:, 0:n], func=AF.Silu, bias=cfw[:, l, 132 + c:133 + c], scale=cfw[:, l, 128 + c:129 + c]), [Z_, SMALL], [O_])
                        tl = list(range(t0 // 128, (t0 + n) // 128))
                        spq.dma(brT[1536 + c * 128:1536 + (c + 1) * 128, t0:t0 + n], o_[:, 0:n], reads=[O_], writes=[BRB[12 + c][i] for i in tl])
                for c in range(4):
                    ws = []
                    for part in range(3):
                        w_, W_ = wa[wi % 6], WA[wi % 6]
                        wi += 1
                        load_w(w_[:], W_, w_in_l[OFF_SC // 128 + part * 4 + c])
                        ws.append((w_, W_))
                    POOL.op(lambda e: e.memset(cfin[:, 0:1], 0.0), [], [CFIN])
                    POOL.op(lambda e: e.memset(cfin[:, 1 + L:2 + L], 0.0), [], [CFIN])
                    for ii, (o, n) in enumerate(nch):
                        t0 = s0 + o
                        pbs = [(psum[3 * (ii % 2) + p_], PSB[3 * (ii % 2) + p_]) for p_ in range(3)]
                        for p_ in range(3):
                            for k in range(KC):
                                mm(pbs[p_][0][:, 0:n], pbs[p_][1], ws[p_][0][:, k, :], [ws[p_][1]], uT[:, k, t0:t0 + n], ut_bufs_k(t0, n, k), k == 0, k == KC - 1)
                        s_, S_ = sg[ii % 2], SG[ii % 2]
                        ACT.op(lambda e: e.copy(bsave[:, o:o + n], pbs[0][0][:, 0:n]), [pbs[0][1]], [BSAVE])
                        ACT.op(lambda e: e.copy(s_[:, 0:n], pbs[2][0][:, 0:n]), [pbs[2][1]], [S_])
                        DVE.op(lambda e: e.tensor_tensor(out=cfin[:, 1 + o:1 + o + n], in0=pbs[1][0][:, 0:n], in1=s_[:, 0:n], op=ALU.mult), [pbs[1][1], S_], [CFIN])
                    p0s(7)
                    w0 = 136 + c * 3
                    DVE.op(lambda e: e.tensor_scalar(out=ysc[:, 0:L], in0=cfin[:, 0:L], scalar1=cfw[:, l, w0:w0 + 1], scalar2=None, op0=ALU.mult), [CFIN, SMALL], [YSC])
                    for kk in (1, 2):
                        DVE.op(lambda e: e.scalar_tensor_tensor(out=ysc[:, 0:L], in0=cfin[:, kk:kk + L], scalar=cfw[:, l, w0 + kk:w0 + kk + 1], in1=ysc[:, 0:L], op0=ALU.mult, op1=ALU.add), [CFIN, SMALL, YSC], [YSC])
                    DVE.op(lambda e: e.tensor_tensor(out=bto[:, 0:L], in0=ysc[:, 0:L], in1=bsave[:, 0:L], op=ALU.mult), [YSC, BSAVE], [BTO])
                    tl = list(range(s0 // 128, (s0 + L) // 128))
                    spq.dma(brT[1024 + c * 128:1024 + (c + 1) * 128, s0:s0 + L], bto[:, 0:L], reads=[BTO], writes=[BRB[8 + c][i] for i in tl])
            p0s(1000)
            C.barrier()

        if dbg and l == 0:
            spq.dma(dbg_br, brT, reads=[b for bb in BRB for b in bb])

        with ExitStack() as ph:
            psum, PSB, psum_b, PSBB = mkps(ph, 6, 2)
            brc = [C.sb([128, KC, 512], BF16, ph) for _ in range(2)]
            BRC = bufs(2)
            NSL = 8
            slab = [C.sb([128, KC, 128], BF16, ph) for _ in range(NSL)]
            SLAB = bufs(NSL)
            sl_i = [0]

            def next_slab(src3):
                i_ = sl_i[0] % NSL
                sl_i[0] += 1
                load_w(slab[i_][:], SLAB[i_], src3)
                return slab[i_], SLAB[i_]
            gt = [C.sb([128, 512], F32, ph) for _ in range(3)]
            GT = bufs(3)
            ma = C.sb([128, 512], F32, ph)
            MA = Buf()
            mb = C.sb([128, 512], F32, ph)
            MB = Buf()
            mT = [C.sb([128, KC, 512], BF16, ph) for _ in range(2)]
            MT = [bufs(KC) for _ in range(2)]
            hold = [C.sb([128, 512], F32, ph) for _ in range(2)]
            HOLD = bufs(2)
            hnew = [C.sb([128, 512], F32, ph) for _ in range(2)]
            HNEW = bufs(2)
            supers = [[(256 + 1024 * sc_ + 512 * ss, 512, 0) for ss in range(2)] for sc_ in range(2)]
            if ctx_full:
                supers.append([(0, 256, 1)])
            hi = 0
            for subs in supers:
                for si, (t0, n, r) in enumerate(subs):
                    tl = list(range(t0 // 128, (t0 + n) // 128))
                    spq.dma(brc[si][:, :, 0:n], brT3[:, :, t0:t0 + n], reads=[BRB[k][i] for k in range(KC) for i in tl], writes=[BRC[si]])
                for j in range(KC):
                    gsl = [next_slab(w_in_l[OFF_GATE // 128 + br * 16 + j]) for br in range(3)]
                    w_o, W_O = next_slab(w_o_in[l, j])
                    for si, (t0, n, r) in enumerate(subs):
                        b_, B_ = brc[si], BRC[si]
                        for br in range(3):
                            for k in range(KC):
                                mm(psum[br][:, 0:n], PSB[br], gsl[br][0][:, k, :], [gsl[br][1]], uT[:, k, t0:t0 + n], ut_bufs_k(t0, n, k), k == 0, k == KC - 1)
                            ACT.op(lambda e: e.activation(out=gt[br][:, 0:n], in_=psum[br][:, 0:n], func=AF.Sigmoid, bias=small[:, l, 32 + br * 16 + j:33 + br * 16 + j], scale=1.0), [PSB[br], SMALL], [GT[br]])
                        for br, (k0, k1) in enumerate(((0, 8), (8, 12), (12, 16))):
                            for k in range(k0, k1):
                                mm(psum[3 + br][:, 0:n], PSB[3 + br], w_o[:, k, :], [W_O], b_[:, k, 0:n], [B_], k == k0, k == k1 - 1)
                        DVE.op(lambda e: e.tensor_tensor(out=ma[:, 0:n], in0=psum[3][:, 0:n], in1=gt[0][:, 0:n], op=ALU.mult), [PSB[3], GT[0]], [MA])
                        DVE.op(lambda e: e.tensor_tensor(out=mb[:, 0:n], in0=psum[4][:, 0:n], in1=gt[1][:, 0:n], op=ALU.mult), [PSB[4], GT[1]], [MB])
                        DVE.op(lambda e: e.tensor_tensor(out=ma[:, 0:n], in0=ma[:, 0:n], in1=mb[:, 0:n], op=ALU.add), [MA, MB], [MA])
                        DVE.op(lambda e: e.tensor_tensor(out=mb[:, 0:n], in0=psum[5][:, 0:n], in1=gt[2][:, 0:n], op=ALU.mult), [PSB[5], GT[2]], [MB])
                        DVE.op(lambda e: e.tensor_tensor(out=mT[si][:, j, 0:n], in0=ma[:, 0:n], in1=mb[:, 0:n], op=ALU.add), [MA, MB], [MT[si][j]])
                for j2 in range(KC):
                    w_m, W_M = next_slab(w_mix[l, j2])
                    for si, (t0, n, r) in enumerate(subs):
                        tl = list(range(t0 // 128, (t0 + n) // 128))
                        ho, HO = hold[hi % 2], HOLD[hi % 2]
                        hn, HN = hnew[hi % 2], HNEW[hi % 2]
                        pb, PB_ = psum[hi % 2], PSB[hi % 2]
                        hi += 1
                        spq.dma(ho[:, 0:n], hT[j2 * 128:(j2 + 1) * 128, t0:t0 + n], reads=htb(tl, j2), writes=[HO])
                        for k in range(KC):
                            mm(pb[:, 0:n], PB_, w_m[:, k, :], [W_M], mT[si][:, k, 0:n], [MT[si][k]], k == 0, k == KC - 1)
                        DVE.op(lambda e: e.scalar_tensor_tensor(out=hn[:, 0:n], in0=pb[:, 0:n], scalar=modT[:, l, 32 + j2, r:r + 1], in1=ho[:, 0:n], op0=ALU.mult, op1=ALU.add), [PB_, HO, MOD[l]], [HN])
                        spq.dma(hT[j2 * 128:(j2 + 1) * 128, t0:t0 + n], hn[:, 0:n], reads=[HN], writes=htb(tl, j2))
            C.barrier()

        phU.close()
        if dbg and l == 0 and not with_moe:
            spq.dma(dbg_h, hT, reads=htb(range(NT)))

        if not with_moe:
            continue

        tiles4 = list(range(NT)) if ctx_full else list(range(2, NT))
        with ExitStack() as ph4:
            sid = C.sb([128, NT, 2], I32, ph4)
            wts = C.sb([128, NT, 2], F32, ph4)
            SIDB = bufs(NT)
            with ExitStack() as ph:
                psum, PSB, psum_b, PSBB = mkps(ph, 8, 0)
                p0g = None
                hcb = [C.sb([128, KC, 256], F32, ph) for _ in range(2)]
                HCB = bufs(2)
                sq = C.sb([128, KC, 256], F32, ph)
                SQ = Buf()
                rstd = [C.sb([128, 256], F32, ph) for _ in range(2)]
                RS = bufs(2)
                tmp = [C.sb([128, 256], F32, ph) for _ in range(2)]
                TMP = bufs(2)
                fT = [C.sb([128, KC, 256], F32, ph) for _ in range(2)]
                FT = bufs(2)
                wrs = C.sb([128, KC, 36], F32, ph)
                rbs = C.sb([1, 36], F32, ph)
                WRS = Buf()
                spq.dma(wrs[:], wr[l].rearrange("(k p) c -> p k c", p=128), writes=[WRS])
                spq.dma(rbs[:], rb[l], writes=[WRS])
                tot = C.sb([128, 32], F32, ph)
                TOT = Buf()
                DVE.op(lambda e: e.memset(tot[:], 0.0), [], [TOT])
                ftok = [C.sb([128, D], BF16, ph) for _ in range(2)]
                FTOK = bufs(2)
                sm_ = [C.sb([128, 512], F32, ph) for _ in range(2)]
                SMB = bufs(2)
                ohb = [C.sb([128, 32], BF16, ph) for _ in range(2)]
                OHB = bufs(2)
                chunks4 = list(range(9)) if ctx_full else list(range(1, 9))
                ti = 0
                for c in chunks4:
                    r = 1 if c == 0 else 0
                    hc, HC = hcb[c % 2], HCB[c % 2]
                    rs_, RS_ = rstd[c % 2], RS[c % 2]
                    f_, F_ = fT[c % 2], FT[c % 2]
                    spq.dma(hc[:], hT3[:, :, c * 256:(c + 1) * 256], reads=htb([2 * c, 2 * c + 1]), writes=[HC])
                    ACT.op(lambda e: e.activation(out=sq[:], in_=hc[:], func=AF.Square), [HC], [SQ])
                    pb, PB_ = psum[4], PSB[4]
                    for k in range(KC):
                        mm(pb[:, 0:256], PB_, ones_f, [CST], sq[:, k, :], [SQ], k == 0, k == KC - 1)
                    ACT.op(lambda e: e.activation(out=rs_[:], in_=pb[:, 0:256], func=AF.Sqrt, bias=eps6, scale=1.0 / D), [PB_], [RS_])
                    DVE.op(lambda e: e.reciprocal(rs_[:], rs_[:]), [RS_], [RS_])
                    for k in range(KC):
                        t_, T_ = tmp[k % 2], TMP[k % 2]
                        DVE.op(lambda e: e.scalar_tensor_tensor(out=t_[:], in0=hc[:, k, :], scalar=col(A2, l, k, r), in1=rs_[:], op0=ALU.mult, op1=ALU.mult), [HC, RS_, MOD[l]], [T_])
                        ACT.op(lambda e: e.activation(out=f_[:, k, :], in_=t_[:], func=AF.Identity, bias=modT[:, l, 48 + k, r:r + 1], scale=1.0), [T_, MOD[l]], [F_])
                    def tile_gen(half, ti, f_=f_, F_=F_, c=c):
                        i = 2 * c + half
                        cs = slice(half * 128, (half + 1) * 128)
                        s_, S_ = sm_[ti % 2], SMB[ti % 2]
                        oh_, OH_ = ohb[ti % 2], OHB[ti % 2]
                        fk, FK = ftok[ti % 2], FTOK[ti % 2]
                        pl, PL_ = psum[5 - half], PSB[5 - half]
                        for k in range(KC):
                            mm(pl[:, 0:36], PL_, f_[:, k, cs], [F_], wrs[:, k, :], [WRS], k == 0, False)
                        mm(pl[:, 0:36], PL_, ones_f[0:1, :], [CST], rbs[0:1, :], [WRS], False, True)
                        lg = s_[:, 0:36]
                        gmax, ngmax, sum4, pg = s_[:, 36:37], s_[:, 37:38], s_[:, 38:39], s_[:, 39:40]
                        gmask, pen, ex4 = s_[:, 40:44], s_[:, 44:48], s_[:, 48:52]
                        lem = s_[:, 64:96]
                        oh1, oh2, lem2 = s_[:, 96:128], s_[:, 128:160], s_[:, 160:192]
                        m1v, m2v, dd, e2, rden = s_[:, 192:193], s_[:, 193:194], s_[:, 194:195], s_[:, 195:196], s_[:, 196:197]
                        rk, tm, oh = s_[:, 200:232], s_[:, 232:264], s_[:, 264:296]
                        sf = s_[:, 296:298]
                        V = lambda fn, rd_=(), wr_=(): DVE.op(fn, [S_] + list(rd_), [S_] + list(wr_))
                        DVE.op(lambda e: e.tensor_copy(lg, pl[:, 0:36]), [PL_], [S_])
                        yield
                        V(lambda e: e.tensor_reduce(out=gmax, in_=s_[:, 0:4], axis=AX.X, op=ALU.max))
                        yield
                        V(lambda e: e.tensor_scalar(out=gmask, in0=s_[:, 0:4], scalar1=gmax, scalar2=None, op0=ALU.is_equal))
                        yield
                        V(lambda e: e.tensor_scalar(out=ngmax, in0=gmax, scalar1=-1.0, scalar2=None, op0=ALU.mult))
                        yield
                        ACT.op(lambda e: e.activation(out=ex4, in_=s_[:, 0:4], func=AF.Exp, bias=ngmax, scale=1.0), [S_], [S_])
                        yield
                        V(lambda e: e.tensor_reduce(out=sum4, in_=ex4, axis=AX.X, op=ALU.add))
                        yield
                        V(lambda e: e.reciprocal(pg, sum4))
                        yield
                        V(lambda e: e.tensor_scalar(out=pen, in0=gmask, scalar1=-1.0, scalar2=BIG, op0=ALU.add, op1=ALU.mult))
                        yield
                        for g in range(4):
                            V(lambda e: e.tensor_scalar(out=s_[:, 64 + g * 8:72 + g * 8], in0=s_[:, 4 + g * 8:12 + g * 8], scalar1=pen[:, g:g + 1], scalar2=None, op0=ALU.add))
                        V(lambda e: e.tensor_reduce(out=m1v, in_=lem, axis=AX.X, op=ALU.max))
                        yield
                        V(lambda e: e.tensor_scalar(out=oh1, in0=lem, scalar1=m1v, scalar2=None, op0=ALU.is_equal))
                        yield
                        V(lambda e: e.scalar_tensor_tensor(out=lem2, in0=oh1, scalar=-BIG, in1=lem, op0=ALU.mult, op1=ALU.add))
                        yield
                        V(lambda e: e.tensor_reduce(out=m2v, in_=lem2, axis=AX.X, op=ALU.max))
                        yield
                        V(lambda e: e.tensor_scalar(out=oh2, in0=lem2, scalar1=m2v, scalar2=None, op0=ALU.is_equal))
                        yield
                        V(lambda e: e.tensor_tensor(out=dd, in0=m2v, in1=m1v, op=ALU.subtract))
                        yield
                        ACT.op(lambda e: e.activation(out=e2, in_=dd, func=AF.Exp), [S_], [S_])
                        yield
                        V(lambda e: e.tensor_scalar(out=rden, in0=e2, scalar1=1.0, scalar2=None, op0=ALU.add))
                        yield
                        V(lambda e: e.reciprocal(rden, rden))
                        yield
                        V(lambda e: e.tensor_tensor(out=wts[:, i, 0:1], in0=pg, in1=rden, op=ALU.mult), [], [SIDB[i]])
                        yield
                        V(lambda e: e.tensor_tensor(out=wts[:, i, 1:2], in0=wts[:, i, 0:1], in1=e2, op=ALU.mult), [SIDB[i]], [SIDB[i]])
                        yield
                        V(lambda e: e.tensor_tensor(out=oh, in0=oh1, in1=oh2, op=ALU.add))
                        yield
                        DVE.op(lambda e: e.tensor_copy(oh_[:], oh), [S_], [OH_])
                        yield
                        pr_, PR_ = psum[3], PSB[3]
                        ro = half * 64
                        mm(pr_[:, ro:ro + 32], PR_, tri_b, [CB], oh_[:], [OH_], True, True)
                        mm(pr_[:, ro + 32:ro + 64], PR_, ones_b, [CB], oh_[:], [OH_], True, True)
                        DVE.op(lambda e: e.tensor_tensor(out=rk, in0=pr_[:, ro:ro + 32], in1=tot[:], op=ALU.add), [PR_, TOT, S_], [S_])
                        yield
                        V(lambda e: e.tensor_scalar(out=rk, in0=rk, scalar1=float(CAP - 1), scalar2=None, op0=ALU.min))
                        yield
                        V(lambda e: e.tensor_tensor(out=rk, in0=rk, in1=ecap_f, op=ALU.add), [CST])
                        yield
                        DVE.op(lambda e: e.tensor_tensor(out=tot[:], in0=tot[:], in1=pr_[:, ro + 32:ro + 64], op=ALU.add), [PR_, TOT], [TOT])
                        yield
                        V(lambda e: e.tensor_tensor(out=tm, in0=rk, in1=oh1, op=ALU.mult))
                        yield
                        V(lambda e: e.tensor_reduce(out=sf[:, 0:1], in_=tm, axis=AX.X, op=ALU.add))
                        yield
                        V(lambda e: e.tensor_tensor(out=tm, in0=rk, in1=oh2, op=ALU.mult))
                        yield
                        V(lambda e: e.tensor_reduce(out=sf[:, 1:2], in_=tm, axis=AX.X, op=ALU.add))
                        yield
                        V(lambda e: e.tensor_copy(sid[:, i, :], sf), [], [SIDB[i]])
                        yield
                        for g in range(4):
                            for kk in range(4):
                                k = g * 4 + kk
                                tr(psum[g % 3][:, kk * 128:(kk + 1) * 128], PSB[g % 3], f_[:, k, cs], [F_], ident_f, kk == 0)
                            E = ACT if g % 2 else DVE
                            if E is ACT:
                                E.op(lambda e: e.copy(fk[:, g * 512:(g + 1) * 512], psum[g % 3][:, :]), [PSB[g % 3]], [FK])
                            else:
                                E.op(lambda e: e.tensor_copy(fk[:, g * 512:(g + 1) * 512], psum[g % 3][:, :]), [PSB[g % 3]], [FK])
                        for kk in range(2):
                            plq._issue(lambda e: e.indirect_dma_start(out=slotbuf[:, :], out_offset=bass.IndirectOffsetOnAxis(ap=sid[:, i, kk:kk + 1], axis=0), in_=fk[:, :], in_offset=None), [FK, SIDB[i]], [SLOTB])
                    gens = [tile_gen(0, ti), tile_gen(1, ti + 1)]
                    ti += 2
                    for _ in range(8):
                        next(gens[0])
                    while gens:
                        for g_ in list(gens):
                            try:
                                next(g_)
                            except StopIteration:
                                gens.remove(g_)
                    if p0g is not None:
                        for _ in range(3):
                            try:
                                next(p0g)
                            except StopIteration:
                                p0g = None
                                break
                if p0g is not None:
                    for _ in p0g:
                        pass
                C.barrier()

            with ExitStack() as ph:
                psum, PSB, psum_b, PSBB = mkps(ph, 6, 2)
                xg = [C.sb([128, D], BF16, ph) for _ in range(NST)]
                XG = bufs(NST)
                xgT2 = [[C.sb([128, KC, 128], BF16, ph) for _ in range(NST)] for _ in range(2)]
                XGT2 = [bufs(NST) for _ in range(2)]
                wgb = [C.sb([128, KC, 512], BF16, ph) for _ in range(4)]
                WGB = bufs(4)
                wdb = [C.sb([128, 8, 512], BF16, ph) for _ in range(4)]
                WDB = bufs(4)
                sgs = [C.sb([128, 512], F32, ph) for _ in range(2)]
                SGS = bufs(2)
                atok = [C.sb([128, EH], BF16, ph) for _ in range(NST)]
                ATOK = bufs(NST)
                aT = [C.sb([128, 8, 128], BF16, ph) for _ in range(NST)]
                AT = bufs(NST)
                ys = [C.sb([128, D], F32, ph) for _ in range(2)]
                YS = bufs(2)

                def load_x(ex):
                    for st in range(NST):
                        r0 = ex * CAP + st * 128
                        spq.dma(xg[st][:], slotbuf[r0:r0 + 128, :], reads=[SLOTB], writes=[XG[st]])

                def load_gu(ex, i2):
                    load_w(wgb[2 * i2][:], WGB[2 * i2], w_gu[l, ex, i2])
                    load_w(wgb[2 * i2 + 1][:], WGB[2 * i2 + 1], w_gu[l, ex, 2 + i2])

                def load_d(ex, n4):
                    load_w(wdb[n4][:], WDB[n4], w_dn[l, ex, n4])

                def prep(ex):
                    xgT, XGT = xgT2[ex % 2], XGT2[ex % 2]
                    for st in range(NST):
                        for g in range(2):
                            for kk in range(8):
                                k = g * 8 + kk
                                tr(psum_b[g][:, kk * 128:(kk + 1) * 128], PSBB[g], xg[st][:, k * 128:(k + 1) * 128], [XG[st]], ident_b, kk == 0)
                            src = psum_b[g][:, :].rearrange("p (a b) -> p a b", a=8)
                            if g == 0:
                                ACT.op(lambda e: e.copy(xgT[st][:, 0:8, :], src), [PSBB[g]], [XGT[st]])
                            else:
                                DVE.op(lambda e: e.tensor_copy(xgT[st][:, 8:16, :], src), [PSBB[g]], [XGT[st]])

                load_x(0)
                load_gu(0, 0)
                load_gu(0, 1)
                for n4 in range(4):
                    load_d(0, n4)
                for ex in range(NE):
                    nxt = ex + 1 < NE
                    xgT, XGT = xgT2[ex % 2], XGT2[ex % 2]
                    prep(ex)
                    if nxt:
                        load_x(ex + 1)
                    for i2 in range(2):
                        wg_, WG_ = wgb[2 * i2], WGB[2 * i2]
                        wu_, WU_ = wgb[2 * i2 + 1], WGB[2 * i2 + 1]
                        for st in range(NST):
                            sp_ = st % 2
                            pg_, PG_ = psum[2 * sp_], PSB[2 * sp_]
                            pu_, PU_ = psum[2 * sp_ + 1], PSB[2 * sp_ + 1]
                            for k in range(KC):
                                mm(pg_[:, :], PG_, xgT[st][:, k, :], [XGT[st]], wg_[:, k, :], [WG_], k == 0, k == KC - 1)
                            for k in range(KC):
                                mm(pu_[:, :], PU_, xgT[st][:, k, :], [XGT[st]], wu_[:, k, :], [WU_], k == 0, k == KC - 1)
                            ACT.op(lambda e: e.activation(out=sgs[sp_][:], in_=pg_[:, :], func=AF.Silu), [PG_], [SGS[sp_]])
                            DVE.op(lambda e: e.tensor_tensor(out=atok[st][:, i2 * 512:(i2 + 1) * 512], in0=pu_[:, :], in1=sgs[sp_][:], op=ALU.mult), [PU_, SGS[sp_]], [ATOK[st]])
                        if nxt:
                            load_gu(ex + 1, i2)
                    for st in range(NST):
                        sp_ = st % 2
                        for kk in range(8):
                            tr(psum_b[sp_][:, kk * 128:(kk + 1) * 128], PSBB[sp_], atok[st][:, kk * 128:(kk + 1) * 128], [ATOK[st]], ident_b, kk == 0)
                        src = psum_b[sp_][:, :].rearrange("p (a b) -> p a b", a=8)
                        if sp_ == 0:
                            ACT.op(lambda e: e.copy(aT[st][:], src), [PSBB[sp_]], [AT[st]])
                        else:
                            DVE.op(lambda e: e.tensor_copy(aT[st][:], src), [PSBB[sp_]], [AT[st]])
                    for st in range(NST):
                        sp_ = st % 2
                        for n4 in range(4):
                            wd_, WD_ = wdb[n4], WDB[n4]
                            pd_, PD_ = psum[4 + (n4 % 2)], PSB[4 + (n4 % 2)]
                            for k in range(8):
                                mm(pd_[:, :], PD_, aT[st][:, k, :], [AT[st]], wd_[:, k, :], [WD_], k == 0, k == 7)
                            if n4 % 2 == 0:
                                ACT.op(lambda e: e.copy(ys[sp_][:, n4 * 512:(n4 + 1) * 512], pd_[:, :]), [PD_], [YS[sp_]])
                            else:
                                DVE.op(lambda e: e.tensor_copy(ys[sp_][:, n4 * 512:(n4 + 1) * 512], pd_[:, :]), [PD_], [YS[sp_]])
                        r0 = ex * CAP + st * 128
                        spq.dma(yslot[r0:r0 + 128, :], ys[sp_][:], reads=[YS[sp_]], writes=[YSLOTB])
                    if nxt:
                        for n4 in range(4):
                            load_d(ex + 1, n4)
                C.barrier()

            with ExitStack() as ph:
                psum, PSB, psum_b, PSBB = mkps(ph, 6, 2)
                r0b = [C.sb([128, D], F32, ph) for _ in range(2)]
                r1b = [C.sb([128, D], F32, ph) for _ in range(2)]
                R0, R1 = bufs(2), bufs(2)
                comb = [C.sb([128, D], F32, ph) for _ in range(2)]
                COMB = bufs(2)
                hold = [C.sb([128, KC, 128], F32, ph) for _ in range(2)]
                HOLD = bufs(2)
                hn = [C.sb([128, KC, 128], F32, ph) for _ in range(2)]
                HN = bufs(2)
                sqf = C.sb([128, KC, 128], F32, ph)
                SQF = Buf()
                rsf = C.sb([128, 128], F32, ph)
                RSF = Buf()
                fin = C.sb([128, KC, 128], F32, ph)
                FIN = Buf()
                otok = [C.sb([128, D], F32, ph) for _ in range(2)]
                OTOK = bufs(2)
                g2row = [C.sb([128, D], F32, ph) for _ in range(2 if ctx_full else 1)]
                G2R = bufs(2)
                zt_ = C.sb([128, 128], F32, ph)
                ZT_ = Buf()
                xbc = [C.sb([128, 128], F32, ph) for _ in range(2)]
                XBC = bufs(2)
                POOL.op(lambda e: e.memset(zt_[:], 0.0), [], [ZT_])
                for r in range(2 if ctx_full else 1):
                    for g in range(4):
                        for kk in range(4):
                            k = g * 4 + kk
                            ACT.op(lambda e: e.activation(out=xbc[k % 2][:], in_=zt_[:], func=AF.Identity, bias=modT[:, l, 80 + k, r:r + 1], scale=1.0), [ZT_, MOD[l]], [XBC[k % 2]])
                            tr(psum[g][:, kk * 128:(kk + 1) * 128], PSB[g], xbc[k % 2][:], [XBC[k % 2]], ident_f, kk == 0)
                        DVE.op(lambda e: e.tensor_copy(g2row[r][:, g * 512:(g + 1) * 512], psum[g][:, :]), [PSB[g]], [G2R[r]])
                for ii, i in enumerate(tiles4):
                    r = 1 if i < 2 else 0
                    p = ii % 2
                    plq._issue(lambda e: e.indirect_dma_start(out=r0b[p][:, :], out_offset=None, in_=yslot[:, :], in_offset=bass.IndirectOffsetOnAxis(ap=sid[:, i, 0:1], axis=0)), [YSLOTB, SIDB[i]], [R0[p]])
                    plq._issue(lambda e: e.indirect_dma_start(out=r1b[p][:, :], out_offset=None, in_=yslot[:, :], in_offset=bass.IndirectOffsetOnAxis(ap=sid[:, i, 1:2], axis=0)), [YSLOTB, SIDB[i]], [R1[p]])
                    spq.dma(hold[p][:], hT3[:, :, i * 128:(i + 1) * 128], reads=htb([i]), writes=[HOLD[p]])
                    DVE.op(lambda e: e.tensor_scalar(out=comb[p][:], in0=r0b[p][:], scalar1=wts[:, i, 0:1], scalar2=None, op0=ALU.mult), [R0[p], SIDB[i]], [COMB[p]])
                    DVE.op(lambda e: e.scalar_tensor_tensor(out=comb[p][:], in0=r1b[p][:], scalar=wts[:, i, 1:2], in1=comb[p][:], op0=ALU.mult, op1=ALU.add), [R1[p], SIDB[i], COMB[p]], [COMB[p]])
                    DVE.op(lambda e: e.tensor_tensor(out=comb[p][:], in0=comb[p][:], in1=g2row[r][:], op=ALU.mult), [COMB[p], G2R[r]], [COMB[p]])
                    for g in range(4):
                        for kk in range(4):
                            k = g * 4 + kk
                            tr(psum[g][:, kk * 128:(kk + 1) * 128], PSB[g], comb[p][:, k * 128:(k + 1) * 128], [COMB[p]], ident_f, kk == 0)
                        src = psum[g][:, :].rearrange("p (a b) -> p a b", a=4)
                        DVE.op(lambda e: e.tensor_tensor(out=hn[p][:, g * 4:(g + 1) * 4, :], in0=src, in1=hold[p][:, g * 4:(g + 1) * 4, :], op=ALU.add), [PSB[g], HOLD[p]], [HN[p]])
                    if not last:
                        spq.dma(hT3[:, :, i * 128:(i + 1) * 128], hn[p][:], reads=[HN[p]], writes=htb([i]))
                    else:
                        ACT.op(lambda e: e.activation(out=sqf[:], in_=hn[p][:], func=AF.Square), [HN[p]], [SQF])
                        for k in range(KC):
                            mm(psum[4][:, 0:128], PSB[4], ones_f, [CST], sqf[:, k, :], [SQF], k == 0, k == KC - 1)
                        ACT.op(lambda e: e.activation(out=rsf[:], in_=psum[4][:, 0:128], func=AF.Sqrt, bias=eps6, scale=1.0 / D), [PSB[4]], [RSF])
                        DVE.op(lambda e: e.reciprocal(rsf[:], rsf[:]), [RSF], [RSF])
                        for k in range(KC):
                            DVE.op(lambda e: e.scalar_tensor_tensor(out=fin[:, k, :], in0=hn[p][:, k, :], scalar=fg[:, k:k + 1], in1=rsf[:], op0=ALU.mult, op1=ALU.mult), [HN[p], RSF, SMALL], [FIN])
                        for g in range(4):
                            for kk in range(4):
                                k = g * 4 + kk
                                tr(psum[g][:, kk * 128:(kk + 1) * 128], PSB[g], fin[:, k, :], [FIN], ident_f, kk == 0)
                            if g % 2:
                                ACT.op(lambda e: e.copy(otok[p][:, g * 512:(g + 1) * 512], psum[g][:, :]), [PSB[g]], [OTOK[p]])
                            else:
                                DVE.op(lambda e: e.tensor_copy(otok[p][:, g * 512:(g + 1) * 512], psum[g][:, :]), [PSB[g]], [OTOK[p]])
                        spq.dma(out[(i - 2) * 128:(i - 1) * 128, :], otok[p][:], reads=[OTOK[p]])
                C.barrier()
        if dbg and l == 0:
            spq.dma(dbg_h, hT, reads=htb(range(NT)))

    C.barrier()
    for t in spq.outstanding() + plq.outstanding():
        SP.wait(t)
    C.es.close()
    return nc


def _consts():
    c = np.zeros((128, 640 + 2 * NLAT), np.float32)
    c[:, 0:128] = np.eye(128, dtype=np.float32)
    c[:, 128:256] = 1.0
    p = np.arange(128)
    c[p ^ 16, 256 + p] = 1.0
    c[:, 384:512] = (p[:, None] < p[None, :]).astype(np.float32)
    c[:, 512:544] = (np.arange(32) * CAP).astype(np.float32)[None, :]
    c[:, 544] = 1e-6
    c[:, 545] = 1e-5
    tok = np.arange(NLAT)
    pos = np.stack([tok // 64, tok % 64], -1).astype(np.float32)
    inv = (10000.0 ** (-np.arange(16, dtype=np.float32) / 16)).astype(np.float32)
    dd = p % 64
    axis = dd // 32
    half = (dd % 32) // 16
    f = dd % 16
    ang = pos[:, axis].T.astype(np.float32) * inv[f][:, None]
    c[:, 640:640 + NLAT] = np.cos(ang)
    sgn = np.where(half == 0, -1.0, 1.0).astype(np.float32)[:, None]
    c[:, 640 + NLAT:] = np.sin(ang) * sgn
    return c


def _colT(v, k):
    return np.ascontiguousarray(np.asarray(v, np.float32).reshape(k, 128).T)


def _tile(w, cw):
    w = np.asarray(w, np.float32)
    lead = w.shape[:-2]
    K_, N_ = w.shape[-2:]
    nl = len(lead)
    w = w.reshape(*lead, K_ // 128, 128, N_ // cw, cw)
    perm = tuple(range(nl)) + (nl + 2, nl + 1, nl + 0, nl + 3)
    return np.ascontiguousarray(w.transpose(perm))


def make_shared(inp, n_layers=DEPTH, with_moe=True):
    L = n_layers
    f = lambda a: np.ascontiguousarray(np.asarray(a, dtype=np.float32))
    m = {}
    m["ada_w"] = _tile(inp["ada_w"][:L], 512)
    m["ada_bT"] = f(np.stack([_colT(inp["ada_b"][l], 96) for l in range(L)]))
    m["n1gT"] = f(np.stack([_colT(inp["norm1_g"][l], KC) for l in range(L)]))
    m["n2gT"] = f(np.stack([_colT(inp["norm2_g"][l], KC) for l in range(L)]))
    m["fgT"] = _colT(inp["final_g"], KC)
    m["w_in"] = _tile(inp["w_in"][:L], 128)
    m["bgT"] = f(np.stack([_colT(inp["b_gate"][l], 48) for l in range(L)]))
    m["dlam"] = f(np.asarray(inp["diff_lambda"][:L]).reshape(L, 256))
    m["sgT"] = f(np.asarray(inp["subln_g"][:L]).reshape(L, 128, 1))
    m["w_o"] = _tile(np.concatenate([inp["w_attn_out"][:L], inp["w_sc_out"][:L], inp["w_cf_out"][:L]], 1), 128)
    m["w_mix"] = _tile(inp["w_mix"][:L], 128)
    m["scwT"] = f(np.stack([np.asarray(inp["sc_conv_w"][l]).reshape(3, 4, 128).transpose(2, 1, 0) for l in range(L)]))
    m["cfwT"] = f(np.stack([np.asarray(inp["cf_dw_w"][l]).reshape(31, 4, 128).transpose(2, 1, 0) for l in range(L)]))
    m["cfbT"] = f(np.stack([_colT(inp["cf_dw_b"][l], 4) for l in range(L)]))
    m["lngT"] = f(np.stack([_colT(inp["cf_ln_g"][l], 4) for l in range(L)]))
    m["lnbT"] = f(np.stack([_colT(inp["cf_ln_b"][l], 4) for l in range(L)]))
    m["consts"] = _consts()
    if with_moe:
        m["wr"] = f(np.concatenate([inp["router_g_w"][:L], inp["router_e_w"][:L]], -1))
        m["rb"] = f(np.concatenate([inp["router_g_b"][:L], inp["router_e_b"][:L]], -1).reshape(L, 1, 36))
        m["w_gu"] = _tile(inp["exp_w_gu"][:L], 512)
        m["w_dn"] = _tile(inp["exp_w_down"][:L], 512)
    return m


def make_in_map(inp, b, n_layers=DEPTH, with_moe=True, shared=None):
    m = dict(shared if shared is not None else make_shared(inp, n_layers, with_moe))
    f = lambda a: np.ascontiguousarray(np.asarray(a, dtype=np.float32))
    m["xin"] = f(np.concatenate([inp["ctx"][b], inp["x"][b]], 0))
    m["cvec"] = f(np.stack([_colT(inp["c"][b], KC), _colT(inp["c_ctx"], KC)], -1))
    return m


def kernel(**inputs):
    inp = {k: np.asarray(v) for k, v in inputs.items()}
    nc = build()
    shared = make_shared(inp)
    in_maps = [make_in_map(inp, b, shared=shared) for b in range(NBATCH)]
    res = run_bass_kernel_spmd(nc, in_maps, core_ids=list(range(NBATCH)))
    return np.stack([np.asarray(r["out"], dtype=np.float32) for r in res.results], 0)
```
